# Optimizing a Trainium2 kernel written in Bass

```python
import math
import jax
import jax.numpy as jnp
from jax import lax
import numpy as np

D_MODEL = 2048
BATCH = 16
SEQ = 2048
DEPTH = 4

GRID_W = 64
CTX_LEN = 256

RWKV_HEADS = 12
RWKV_HEAD = 64
RWKV_W = RWKV_HEADS * RWKV_HEAD
DECAY_LORA = 64
AAA_LORA = 64
GATE_LORA = 128
RWKV_GN_EPS = 64e-5

DIFF_HEADS = 6
DIFF_DK = 64
DIFF_DV = 2 * DIFF_DK
DIFF_W = DIFF_HEADS * DIFF_DV
DIFF_SCALE = DIFF_DK ** -0.5

WIN_Q_HEADS = 8
WIN_KV_HEADS = 2
WIN_GROUP = WIN_Q_HEADS // WIN_KV_HEADS
WIN_HEAD = 64
WIN_W = WIN_Q_HEADS * WIN_HEAD
WIN_KV_W = WIN_KV_HEADS * WIN_HEAD
WIN_SCALE = WIN_HEAD ** -0.5
WINDOW = 128
QBLK = WINDOW

MIX_W = RWKV_W + DIFF_W + WIN_W
N_BRANCH = 3

ROPE_BASE = 10000.0
ROPE_AX_FREQS = 16

RWKV_IN = 3 * RWKV_W + 2 * DECAY_LORA + 2 * AAA_LORA + GATE_LORA
DIFF_IN = 3 * DIFF_W
WIN_IN = WIN_W + 2 * WIN_KV_W
DIFF_OFF = RWKV_IN
WIN_OFF = DIFF_OFF + DIFF_IN
GATE_OFF = WIN_OFF + WIN_IN
N_IN = GATE_OFF + N_BRANCH * D_MODEL

N_GROUPS = 4
EXPERTS_PER_GROUP = 8
N_EXPERTS = N_GROUPS * EXPERTS_PER_GROUP
EXPERT_TOP_K = 2
D_EXPERT = D_MODEL // 4
MOE_BLK = 256

DN_ALPHA = (2 * DEPTH) ** 0.25
DN_BETA = (8 * DEPTH) ** -0.25
ADA_EPS = 1e-6
LN_EPS = 1e-5
NEG_INF = -1e30

kernel_name = "hybrid_rwkv7_diffattn_swa_hmoe_dit"


def layer_norm(x, g, b, eps):
    xf = x.astype(jnp.float32)
    mu = jnp.mean(xf, axis=-1, keepdims=True)
    var = jnp.mean(jnp.square(xf - mu), axis=-1, keepdims=True)
    y = (xf - mu) * lax.rsqrt(var + eps)
    if g is not None:
        y = y * g.astype(jnp.float32) + b.astype(jnp.float32)
    return y.astype(x.dtype)


def modulate(x, shift, scale):
    return layer_norm(x, None, None, ADA_EPS) * (1 + scale) + shift


def axial_rope(x, rope):
    cos, sin = rope
    xs = x.reshape(x.shape[:-1] + (2, 2, ROPE_AX_FREQS)).astype(jnp.float32)
    x1 = xs[..., 0, :]
    x2 = xs[..., 1, :]
    out = jnp.stack([x1 * cos - x2 * sin, x1 * sin + x2 * cos], axis=-2)
    return out.reshape(x.shape).astype(x.dtype)


def centred_shift(p, mu_prev, mu_next):
    zero = jnp.zeros_like(p[:, :1])
    prev = jnp.concatenate([zero, p[:, :-1]], axis=1)
    nxt = jnp.concatenate([p[:, 1:], zero], axis=1)
    return p + mu_prev * (prev - p) + mu_next * (nxt - p)


def rwkv_features(p, mu, w0, w_up, a0, a_up, g_up, kvec):
    B, L, _ = p.shape
    p = centred_shift(p, mu[0], mu[1])
    r = p[..., :RWKV_W]
    k = p[..., RWKV_W:2 * RWKV_W]
    v = p[..., 2 * RWKV_W:3 * RWKV_W]
    o = 3 * RWKV_W
    wd = p[..., o:o + 2 * DECAY_LORA].reshape(B, L, 2, DECAY_LORA)
    o += 2 * DECAY_LORA
    ad = p[..., o:o + 2 * AAA_LORA].reshape(B, L, 2, AAA_LORA)
    o += 2 * AAA_LORA
    gd = p[..., o:o + GATE_LORA]
    w_log = (w0 + jnp.einsum('bldr,drc->bldc', jnp.tanh(wd), w_up)).astype(jnp.float32)
    decay = jnp.exp(-jnp.exp(-jax.nn.softplus(-w_log) - 0.5))
    a = jax.nn.sigmoid((a0 + jnp.einsum('bldr,drc->bldc', ad, a_up)).astype(jnp.float32))
    g = jax.nn.sigmoid(gd) @ g_up
    kv = kvec.astype(jnp.float32)
    k_k, k_a = kv[0], kv[1]
    kf = k.astype(jnp.float32)
    kk = (kf * k_k).reshape(B, L, RWKV_HEADS, RWKV_HEAD)
    kk = kk / jnp.maximum(jnp.sqrt(jnp.sum(kk * kk, axis=-1, keepdims=True)), 1e-12)
    kk_flat = kk.reshape(B, L, RWKV_W)
    k_dir = kf[:, :, None] * (1 + (a - 1) * k_a)
    b_dir = kk_flat[:, :, None] * a
    heads = lambda t: t.reshape(t.shape[:-1] + (RWKV_HEADS, RWKV_HEAD))
    return (heads(r.astype(jnp.float32)), heads(v.astype(jnp.float32)), g,
            heads(decay), kk, heads(k_dir), heads(b_dir))


def rwkv_scan(s0, r, decay, kk, b, k, v, reverse):
    xs = tuple(jnp.moveaxis(t, 1, 0) for t in (r, decay, kk, b, k, v))

    def step(s, inp):
        r_t, w_t, kk_t, b_t, k_t, v_t = inp
        sa = jnp.einsum('bhij,bhj->bhi', s, -kk_t)
        s = (s * w_t[:, :, None, :] + sa[..., None] * b_t[:, :, None, :]
             + v_t[..., None] * k_t[:, :, None, :])
        return s, jnp.einsum('bhij,bhj->bhi', s, r_t)

    s_fin, ys = lax.scan(step, s0, xs, reverse=reverse)
    return s_fin, jnp.moveaxis(ys, 0, 1)


def rwkv_readout(y, r, k_dir, v, g, r_k, lnx):
    B, L, H, N = y.shape
    mu = jnp.mean(y, axis=-1, keepdims=True)
    var = jnp.mean(jnp.square(y - mu), axis=-1, keepdims=True)
    lf = lnx.astype(jnp.float32)
    yn = (y - mu) * lax.rsqrt(var + RWKV_GN_EPS) * lf[0].reshape(H, N) + lf[1].reshape(H, N)
    rk = r_k.astype(jnp.float32).reshape(H, N)
    bonus = jnp.sum(r[:, :, None] * k_dir * rk, axis=(2, 4))[..., None]
    out = (yn + bonus * v).reshape(B, L, H * N) * g.astype(jnp.float32)
    return out.astype(g.dtype)


def rwkv_bidir(f_l, f_c, r_k, lnx, with_ctx):
    r_l, v_l, g_l, dec_l, kk_l, kd_l, bd_l = f_l
    r_c, v_c, g_c, dec_c, kk_c, kd_c, bd_c = f_c
    B, _, H, N = r_l.shape
    s0 = jnp.zeros((B, H, N, N), jnp.float32)
    ys_l, ys_c = [], []
    for d in range(2):
        rev = d == 1
        s_ctx, yc = rwkv_scan(s0, r_c, dec_c[:, :, d], kk_c, bd_c[:, :, d], kd_c[:, :, d], v_c, rev)
        _, yl = rwkv_scan(s_ctx, r_l, dec_l[:, :, d], kk_l, bd_l[:, :, d], kd_l[:, :, d], v_l, rev)
        ys_l.append(yl)
        ys_c.append(yc)
    out_l = rwkv_readout(ys_l[0] + ys_l[1], r_l, kd_l, v_l, g_l, r_k, lnx)
    out_c = rwkv_readout(ys_c[0] + ys_c[1], r_c, kd_c, v_c, g_c, r_k, lnx) if with_ctx else None
    return out_l, out_c


def diff_attention(p_l, p_c, lam_vecs, subln_g, lam_init, rope, with_ctx):
    def split(p):
        B, L, _ = p.shape
        q = p[..., :DIFF_W].reshape(B, L, DIFF_HEADS, 2, DIFF_DK).transpose(0, 2, 3, 1, 4)
        k = p[..., DIFF_W:2 * DIFF_W].reshape(B, L, DIFF_HEADS, 2, DIFF_DK).transpose(0, 2, 3, 1, 4)
        v = p[..., 2 * DIFF_W:].reshape(B, L, DIFF_HEADS, DIFF_DV).transpose(0, 2, 1, 3)
        return q, k, v

    q_l, k_l, v_l = split(p_l)
    q_c, k_c, v_c = split(p_c)
    q_l = axial_rope(q_l, rope)
    k_l = axial_rope(k_l, rope)
    lf = lam_vecs.astype(jnp.float32)
    lam = jnp.exp(jnp.sum(lf[0] * lf[1])) - jnp.exp(jnp.sum(lf[2] * lf[3])) + lam_init

    def attend(q, keys, vals):
        s = jnp.einsum('bhmqd,bhmkd->bhmqk', q, keys).astype(jnp.float32) * DIFF_SCALE
        pr = jax.nn.softmax(s, axis=-1)
        a = pr[:, :, 0] - lam * pr[:, :, 1]
        return jnp.einsum('bhqk,bhkd->bhqd', a.astype(vals.dtype), vals)

    def finish(o):
        B, H, L, _ = o.shape
        of = o.astype(jnp.float32)
        of = of * lax.rsqrt(jnp.mean(of * of, axis=-1, keepdims=True) + 1e-5)
        of = of * subln_g.astype(jnp.float32) * (1 - lam_init)
        return of.transpose(0, 2, 1, 3).reshape(B, L, H * DIFF_DV).astype(o.dtype)

    keys = jnp.concatenate([k_c, k_l], axis=3)
    vals = jnp.concatenate([v_c, v_l], axis=2)
    B, H, _, S, dk = q_l.shape
    nq = S // QBLK
    qb = q_l.reshape(B, H, 2, nq, QBLK, dk).transpose(3, 0, 1, 2, 4, 5)
    o_l = lax.map(lambda q: attend(q, keys, vals), qb)
    o_l = o_l.transpose(1, 2, 0, 3, 4).reshape(B, H, S, DIFF_DV)
    out_c = finish(attend(q_c, k_c, v_c)) if with_ctx else None
    return finish(o_l), out_c


def sink_attend(q, keys, vals, sink_hg, mask):
    s = jnp.einsum('bhgqd,bhkd->bhgqk', q, keys).astype(jnp.float32) * WIN_SCALE
    if mask is not None:
        s = jnp.where(mask, s, NEG_INF)
    sink = jnp.broadcast_to(sink_hg[None, :, :, None, None], s.shape[:-1] + (1,))
    pr = jax.nn.softmax(jnp.concatenate([s, sink], axis=-1), axis=-1)[..., :-1]
    return jnp.einsum('bhgqk,bhkd->bhgqd', pr.astype(vals.dtype), vals)


def window_attention(p_l, p_c, sink, rope, with_ctx):
    def split(p):
        B, L, _ = p.shape
        q = p[..., :WIN_W].reshape(B, L, WIN_KV_HEADS, WIN_GROUP, WIN_HEAD).transpose(0, 2, 3, 1, 4)
        k = p[..., WIN_W:WIN_W + WIN_KV_W].reshape(B, L, WIN_KV_HEADS, WIN_HEAD).transpose(0, 2, 1, 3)
        v = p[..., WIN_W + WIN_KV_W:].reshape(B, L, WIN_KV_HEADS, WIN_HEAD).transpose(0, 2, 1, 3)
        return q, k, v

    q_l, k_l, v_l = split(p_l)
    q_c, k_c, v_c = split(p_c)
    q_l = axial_rope(q_l, rope)
    k_l = axial_rope(k_l, rope)
    sink_hg = sink.astype(jnp.float32).reshape(WIN_KV_HEADS, WIN_GROUP)
    B, Hkv, G, S, dh = q_l.shape
    C = k_c.shape[2]
    nb = S // QBLK
    qb = jnp.moveaxis(q_l.reshape(B, Hkv, G, nb, QBLK, dh), 3, 0)

    def band(t):
        tp = jnp.pad(t, ((0, 0), (0, 0), (QBLK, QBLK), (0, 0))).reshape(B, Hkv, nb + 2, QBLK, dh)
        tw = jnp.concatenate([tp[:, :, :-2], tp[:, :, 1:-1], tp[:, :, 2:]], axis=3)
        return jnp.moveaxis(tw, 2, 0)

    blk_ids = jnp.arange(nb)[:, None, None] * QBLK
    qpos = blk_ids + jnp.arange(QBLK)[None, :, None]
    kpos = blk_ids - QBLK + jnp.arange(3 * QBLK)[None, None, :]
    valid = (jnp.abs(qpos - kpos) <= WINDOW) & (kpos >= 0) & (kpos < S)
    valid = jnp.concatenate([valid, jnp.ones((nb, QBLK, C), bool)], axis=-1)

    def blk(args):
        q, kw, vw, m = args
        return sink_attend(q, jnp.concatenate([kw, k_c], axis=2),
                           jnp.concatenate([vw, v_c], axis=2), sink_hg, m)

    o = lax.map(blk, (qb, band(k_l), band(v_l), valid))
    out_l = o.transpose(1, 0, 4, 2, 3, 5).reshape(B, S, WIN_W)
    out_c = None
    if with_ctx:
        o_c = sink_attend(q_c, k_c, v_c, sink_hg, None)
        out_c = o_c.transpose(0, 3, 1, 2, 4).reshape(B, C, WIN_W)
    return out_l, out_c


def merge(p_gate, y_a, y_b, y_c, w_branch, w_out):
    g = jax.nn.sigmoid(p_gate.reshape(p_gate.shape[:-1] + (N_BRANCH, D_MODEL)))
    z = (g[..., 0, :] * (y_a @ w_branch[:RWKV_W])
         + g[..., 1, :] * (y_b @ w_branch[RWKV_W:RWKV_W + DIFF_W])
         + g[..., 2, :] * (y_c @ w_branch[RWKV_W + DIFF_W:]))
    return z @ w_out


def token_mixer(u_l, u_c, w_in, mu, w0, w_up, a0, a_up, g_up, kvec, lnx,
                lam_vecs, subln_g, sink, w_branch, w_out, rope, lam_init, with_ctx):
    p_l = u_l @ w_in
    p_c = u_c @ w_in
    f_l = rwkv_features(p_l[..., :RWKV_IN], mu, w0, w_up, a0, a_up, g_up, kvec)
    f_c = rwkv_features(p_c[..., :RWKV_IN], mu, w0, w_up, a0, a_up, g_up, kvec)
    ya_l, ya_c = rwkv_bidir(f_l, f_c, kvec[2], lnx, with_ctx)
    yb_l, yb_c = diff_attention(p_l[..., DIFF_OFF:WIN_OFF], p_c[..., DIFF_OFF:WIN_OFF],
                                lam_vecs, subln_g, lam_init, rope, with_ctx)
    yc_l, yc_c = window_attention(p_l[..., WIN_OFF:GATE_OFF], p_c[..., WIN_OFF:GATE_OFF],
                                  sink, rope, with_ctx)
    out_l = merge(p_l[..., GATE_OFF:], ya_l, yb_l, yc_l, w_branch, w_out)
    out_c = merge(p_c[..., GATE_OFF:], ya_c, yb_c, yc_c, w_branch, w_out) if with_ctx else None
    return out_l, out_c


def expert_dispatch(u, experts, gates, w1, w3, w2):
    T, D = u.shape
    K = experts.shape[1]
    A = T * K
    E = w1.shape[0]
    nb = -(-A // MOE_BLK) + E
    e_flat = experts.reshape(A)
    tok = jnp.arange(A, dtype=jnp.int32) // K
    g_flat = gates.reshape(A)
    order = jnp.argsort(e_flat)
    e_s = e_flat[order]
    counts = jnp.bincount(e_flat, length=E)
    padded = (counts + MOE_BLK - 1) // MOE_BLK * MOE_BLK
    pad_end = jnp.cumsum(padded)
    pad_start = pad_end - padded
    start = jnp.cumsum(counts) - counts
    dest = pad_start[e_s] + jnp.arange(A, dtype=jnp.int32) - start[e_s]
    slot_tok = jnp.full((nb * MOE_BLK,), T, jnp.int32).at[dest].set(tok[order])
    slot_gate = jnp.zeros((nb * MOE_BLK,), u.dtype).at[dest].set(g_flat[order].astype(u.dtype))
    blk_expert = jnp.minimum(jnp.searchsorted(pad_end, jnp.arange(nb) * MOE_BLK, side='right'), E - 1)
    u_pad = jnp.concatenate([u, jnp.zeros((1, D), u.dtype)], axis=0)

    def run_block(args):
        t_idx, gw, e = args
        xb = u_pad[t_idx]
        h = jax.nn.silu(xb @ w1[e]) * (xb @ w3[e])
        return (h @ w2[e]) * gw[:, None]

    y_slots = lax.map(run_block, (slot_tok.reshape(nb, MOE_BLK), slot_gate.reshape(nb, MOE_BLK), blk_expert))
    y = jax.ops.segment_sum(y_slots.reshape(-1, D), slot_tok, num_segments=T + 1)
    return y[:T]


def hier_moe(u, w_rg, b_rg, w_re, b_re, w1, w3, w2):
    T = u.shape[0]
    pg = jax.nn.softmax((u @ w_rg).astype(jnp.float32) + b_rg.astype(jnp.float32), axis=-1)
    pg_top, g_idx = lax.top_k(pg, 1)
    le = ((u @ w_re).astype(jnp.float32) + b_re.astype(jnp.float32)).reshape(T, N_GROUPS, EXPERTS_PER_GROUP)
    le_sel = jnp.take_along_axis(le, g_idx[:, :, None], axis=1)[:, 0]
    pe_top, e_loc = lax.top_k(jax.nn.softmax(le_sel, axis=-1), EXPERT_TOP_K)
    gates = pg_top * pe_top / jnp.sum(pe_top, axis=-1, keepdims=True)
    experts = g_idx * EXPERTS_PER_GROUP + e_loc
    return expert_dispatch(u, experts, gates, w1, w3, w2)


def setup_inputs(seed: int = 0) -> dict:
    key = jax.random.key(seed)
    ks = jax.random.split(key, 32)
    f32 = jnp.float32
    nrm = lambda k, shape, s: jax.random.normal(k, shape, f32) * s
    L, D, E, F = DEPTH, D_MODEL, N_EXPERTS, D_EXPERT
    col_scale = np.ones((N_IN,), np.float32)
    col_scale[2 * RWKV_W:3 * RWKV_W] = DN_BETA
    col_scale[DIFF_OFF + 2 * DIFF_W:DIFF_OFF + 3 * DIFF_W] = DN_BETA
    col_scale[WIN_OFF + WIN_W + WIN_KV_W:GATE_OFF] = DN_BETA
    kvec_mean = jnp.array([0.85, 1.0, 0.0], f32)[None, :, None]
    kvec_std = jnp.array([0.02, 0.02, 0.1], f32)[None, :, None]
    return {
        "x": nrm(ks[0], (BATCH, SEQ, D), 1.0),
        "c": nrm(ks[1], (BATCH, D), 1.0),
        "ctx": nrm(ks[2], (BATCH, CTX_LEN, D), 1.0),
        "c_ctx": nrm(ks[3], (D,), 1.0),
        "w_mod": nrm(ks[4], (L, D, 6 * D), 0.5 * D ** -0.5),
        "b_mod": nrm(ks[5], (L, 6 * D), 0.02),
        "w_in": nrm(ks[6], (L, D, N_IN), D ** -0.5) * jnp.asarray(col_scale),
        "rwkv_mu": jax.random.uniform(ks[7], (L, 2, RWKV_IN), f32, 0.0, 0.5),
        "rwkv_w0": jax.random.uniform(ks[8], (L, 2, RWKV_W), f32, -6.0, -1.0),
        "rwkv_w_up": nrm(ks[9], (L, 2, DECAY_LORA, RWKV_W), 0.1 * DECAY_LORA ** -0.5),
        "rwkv_a0": nrm(ks[10], (L, 2, RWKV_W), 0.1),
        "rwkv_a_up": nrm(ks[11], (L, 2, AAA_LORA, RWKV_W), 0.5 * AAA_LORA ** -0.5),
        "rwkv_g_up": nrm(ks[12], (L, GATE_LORA, RWKV_W), GATE_LORA ** -0.5),
        "rwkv_kvec": kvec_mean + kvec_std * jax.random.normal(ks[13], (L, 3, RWKV_W), f32),
        "rwkv_lnx": jnp.array([1.0, 0.0], f32)[None, :, None] + nrm(ks[14], (L, 2, RWKV_W), 0.02),
        "diff_lam": nrm(ks[15], (L, 4, DIFF_DK), 0.1),
        "diff_subln": 1.0 + nrm(ks[16], (L, DIFF_DV), 0.02),
        "win_sink": nrm(ks[17], (L, WIN_Q_HEADS), 0.5),
        "w_branch": nrm(ks[18], (L, MIX_W, D), (MIX_W / N_BRANCH) ** -0.5),
        "w_out": nrm(ks[19], (L, D, D), D ** -0.5 * DN_BETA),
        "ln_g": 1.0 + nrm(ks[20], (L, 2, D), 0.02),
        "ln_b": nrm(ks[21], (L, 2, D), 0.02),
        "w_rg": nrm(ks[22], (L, D, N_GROUPS), D ** -0.5),
        "b_rg": nrm(ks[23], (L, N_GROUPS), 0.01),
        "w_re": nrm(ks[24], (L, D, E), D ** -0.5),
        "b_re": nrm(ks[25], (L, E), 0.01),
        "w1": nrm(ks[26], (L, E, D, F), D ** -0.5),
        "w3": nrm(ks[27], (L, E, D, F), D ** -0.5),
        "w2": nrm(ks[28], (L, E, F, D), F ** -0.5 * DN_BETA),
    }


def reference(x, c, ctx, c_ctx, w_mod, b_mod, w_in, rwkv_mu, rwkv_w0, rwkv_w_up, rwkv_a0,
              rwkv_a_up, rwkv_g_up, rwkv_kvec, rwkv_lnx, diff_lam, diff_subln, win_sink,
              w_branch, w_out, ln_g, ln_b, w_rg, b_rg, w_re, b_re, w1, w3, w2):
    B, S, D = x.shape
    rows = S // GRID_W
    row = jnp.repeat(jnp.arange(rows), GRID_W).astype(jnp.float32)
    col = (jnp.arange(rows * GRID_W) % GRID_W).astype(jnp.float32)
    inv = ROPE_BASE ** (-jnp.arange(ROPE_AX_FREQS, dtype=jnp.float32) / ROPE_AX_FREQS)
    ang = jnp.stack([row[:, None] * inv, col[:, None] * inv], axis=1)
    rope = (jnp.cos(ang), jnp.sin(ang))
    xc = ctx
    for i in range(DEPTH):
        last = i == DEPTH - 1
        lam_init = 0.8 - 0.6 * math.exp(-0.3 * i)
        mod_l = jnp.split((jax.nn.silu(c) @ w_mod[i] + b_mod[i])[:, None, :], 6, axis=-1)
        mod_c = jnp.split(jax.nn.silu(c_ctx) @ w_mod[i] + b_mod[i], 6, axis=-1)
        u_l = modulate(x, mod_l[0], mod_l[1])
        u_c = modulate(xc, mod_c[0], mod_c[1])
        m_l, m_c = token_mixer(u_l, u_c, w_in[i], rwkv_mu[i], rwkv_w0[i], rwkv_w_up[i], rwkv_a0[i],
                               rwkv_a_up[i], rwkv_g_up[i], rwkv_kvec[i], rwkv_lnx[i], diff_lam[i],
                               diff_subln[i], win_sink[i], w_branch[i], w_out[i], rope, lam_init,
                               not last)
        x = layer_norm(DN_ALPHA * x + mod_l[2] * m_l, ln_g[i, 0], ln_b[i, 0], LN_EPS)
        v_l = modulate(x, mod_l[3], mod_l[4])
        if last:
            y_l = hier_moe(v_l.reshape(-1, D), w_rg[i], b_rg[i], w_re[i], b_re[i],
                           w1[i], w3[i], w2[i]).reshape(B, S, D)
        else:
            xc = layer_norm(DN_ALPHA * xc + mod_c[2] * m_c, ln_g[i, 0], ln_b[i, 0], LN_EPS)
            v_c = modulate(xc, mod_c[3], mod_c[4])
            y = hier_moe(jnp.concatenate([v_l.reshape(-1, D), v_c.reshape(-1, D)], axis=0),
                         w_rg[i], b_rg[i], w_re[i], b_re[i], w1[i], w3[i], w2[i])
            y_l = y[:B * S].reshape(B, S, D)
            y_c = y[B * S:].reshape(xc.shape)
            xc = layer_norm(DN_ALPHA * xc + mod_c[5] * y_c, ln_g[i, 1], ln_b[i, 1], LN_EPS)
        x = layer_norm(DN_ALPHA * x + mod_l[5] * y_l, ln_g[i, 1], ln_b[i, 1], LN_EPS)
    return x
```

```python
import math
import numpy as np
import ml_dtypes
import concourse.bass as bass
import concourse.mybir as mybir
from concourse.bass_utils import run_bass_kernel_spmd

F32 = mybir.dt.float32
BF16 = mybir.dt.bfloat16
I32 = mybir.dt.int32
U32 = mybir.dt.uint32
AF = mybir.ActivationFunctionType
ALU = mybir.AluOpType
AX = mybir.AxisListType

D = 2048
KD = 16
RW = 768
DIFF_W = 768
WIN_W = 512
WIN_KV_W = 128
RWKV_IN = 2688
DIFF_OFF = 2688
WIN_OFF = DIFF_OFF + 2304
GATE_OFF = WIN_OFF + 768
N_IN = GATE_OFF + 3 * D
DEPTH_FULL = 4
DN_ALPHA = (2 * DEPTH_FULL) ** 0.25
ADA_EPS = 1e-6
LN_EPS = 1e-5


def _rope_partner(cols):
    cols = np.asarray(cols).reshape(-1, 64)
    d = np.arange(64)
    return cols[:, d ^ 16].reshape(-1)


def ext_cols():
    sec = {}
    out = []

    def add(name, c):
        c = np.asarray(c, dtype=np.int64)
        assert len(c) % 128 == 0, name
        sec[name] = (len(out_flat()), len(c))
        out.append(c)

    def out_flat():
        return np.concatenate(out) if out else np.zeros((0,), np.int64)

    add("rw", np.arange(0, RWKV_IN))
    dq = DIFF_OFF + np.arange(0, DIFF_W)
    dk = DIFF_OFF + DIFF_W + np.arange(0, DIFF_W)
    dv = DIFF_OFF + 2 * DIFF_W + np.arange(0, DIFF_W)
    add("dq", dq)
    add("dqp", _rope_partner(dq))
    add("dk", dk)
    add("dkp", _rope_partner(dk))
    add("dv", dv)
    wq = []
    for g in range(4):
        for hkv in range(2):
            wq.append(WIN_OFF + hkv * 256 + g * 64 + np.arange(64))
    wq = np.concatenate(wq)
    add("wq", wq)
    add("wqp", _rope_partner(wq))
    wk = WIN_OFF + WIN_W + np.arange(0, WIN_KV_W)
    add("wk", wk)
    add("wkp", _rope_partner(wk))
    add("wv", WIN_OFF + WIN_W + WIN_KV_W + np.arange(0, WIN_KV_W))
    add("gate", GATE_OFF + np.arange(0, 3 * D))
    return out_flat(), sec


EXT_COLS, EXT_SEC = ext_cols()
N_EXT = len(EXT_COLS)
NT_EXT = N_EXT // 128


class Cfg:
    def __init__(self, ncores=8, nb=2, S=2048, C=256, depth=4, dump=(), stop_after=None, dbg=99, cap=768):
        self.dbg = dbg
        self.cap = cap
        self.ncores = ncores
        self.nb = nb
        self.S = S
        self.C = C
        self.L = S + C
        self.depth = depth
        self.dump = tuple(dump)
        self.stop_after = stop_after
        self.NT = self.L // 128


class Buf:
    __slots__ = ("t", "last_w", "readers", "name", "subs")

    def __init__(self, t, name=""):
        self.t = t
        self.last_w = None
        self.readers = {}
        self.name = name
        self.subs = {}

    def sub(self, key):
        b = self.subs.get(key)
        if b is None:
            b = Buf(self.t, f"{self.name}.{key}")
            self.subs[key] = b
        return b

    def __getitem__(self, idx):
        return self.t[idx]

    def ap(self):
        return self.t.ap()


class KB:
    def __init__(self, nc, ndma=32):
        self.nc = nc
        self.E = {"pe": nc.tensor, "act": nc.scalar, "dve": nc.vector, "pool": nc.gpsimd, "sp": nc.sync}
        self.sem = {e: nc.alloc_semaphore(f"s_{e}") for e in self.E}
        self.cnt = {e: 0 for e in self.E}
        self.seen = {e: {} for e in self.E}
        self.ndma = ndma
        self.dsem = [nc.alloc_semaphore(f"d{i}") for i in range(ndma)]
        self.dcnt = [0] * ndma
        self.dnext = 0
        self.ninst = 0

    def sb(self, name, shape, dtype=F32):
        return Buf(self.nc.alloc_sbuf_tensor(name, list(shape), dtype), name)

    def psum(self, name, shape, dtype=F32):
        return Buf(self.nc.alloc_psum_tensor(name, list(shape), dtype), name)

    def dram(self, name, shape, dtype=F32, kind="Internal"):
        return Buf(self.nc.dram_tensor(name, list(shape), dtype, kind=kind), name)

    def _semobj(self, key):
        return self.sem[key] if isinstance(key, str) else self.dsem[key]

    def _wait(self, eng, key, val):
        if eng == "pe" and key == "pe":
            return
        if self.seen[eng].get(key, 0) >= val:
            return
        self.E[eng].wait_ge(self._semobj(key), val)
        self.seen[eng][key] = val
        self.ninst += 1

    def _deps(self, eng, reads, writes):
        for r in reads:
            if r.last_w is not None:
                self._wait(eng, *r.last_w)
        for w in writes:
            if w.last_w is not None:
                self._wait(eng, *w.last_w)
            for k, v in w.readers.items():
                self._wait(eng, k, v)

    def op(self, eng, fn, reads, writes):
        self._deps(eng, reads, writes)
        inst = fn(self.E[eng])
        self.cnt[eng] += 1
        c = self.cnt[eng]
        inst.then_inc(self.sem[eng], 1)
        self.ninst += 1
        for r in reads:
            if r.readers.get(eng, 0) < c:
                r.readers[eng] = c
        for w in writes:
            w.last_w = (eng, c)
            w.readers = {}
        return inst

    def _dma_common(self, q, reads, writes, emit):
        self._deps(q, reads, writes)
        s = self.dnext
        self.dnext = (s + 1) % self.ndma
        if self.dcnt[s] > 0:
            self._wait(q, s, 16 * self.dcnt[s])
        self.dcnt[s] += 1
        v = 16 * self.dcnt[s]
        emit().then_inc(self.dsem[s], 16)
        self.ninst += 1
        for r in reads:
            r.readers[s] = v
        for w in writes:
            w.last_w = (s, v)
            w.readers = {}

    def dma(self, q, out_ap, in_ap, reads, writes, **kw):
        self._dma_common(q, reads, writes, lambda: self.E[q].dma_start(out=out_ap, in_=in_ap, **kw))

    def idma(self, out_ap, out_off, in_ap, in_off, reads, writes, **kw):
        self._dma_common("pool", reads, writes,
                         lambda: self.nc.gpsimd.indirect_dma_start(out=out_ap, out_offset=out_off, in_=in_ap,
                                                                   in_offset=in_off, **kw))

    def barrier(self):
        for e in self.E:
            for e2 in self.E:
                if e2 != e and self.cnt[e2] > 0:
                    self._wait(e, e2, self.cnt[e2])
            for s in range(self.ndma):
                if self.dcnt[s] > 0:
                    self._wait(e, s, 16 * self.dcnt[s])

    def mm(self, out_ap, lhsT, rhs, start, stop, reads, writes, **kw):
        return self.op("pe", lambda e: e.matmul(out_ap, lhsT, rhs, start=start, stop=stop, **kw), reads, writes)

    def tr(self, out_ap, in_ap, ident_ap, reads, writes):
        return self.op("pe", lambda e: e.transpose(out_ap, in_ap, ident_ap), reads, writes)

    def act(self, out_ap, in_ap, func, reads, writes, **kw):
        return self.op("act", lambda e: e.activation(out_ap, in_ap, func, **kw), reads, writes)

    def copy(self, eng, out_ap, in_ap, reads, writes):
        if eng == "act":
            return self.op("act", lambda e: e.copy(out_ap, in_ap), reads, writes)
        return self.op(eng, lambda e: e.tensor_copy(out_ap, in_ap), reads, writes)

    def tt(self, eng, out_ap, a, b, op, reads, writes):
        return self.op(eng, lambda e: e.tensor_tensor(out_ap, a, b, op), reads, writes)

    def ts(self, eng, out_ap, a, s1, s2, op0, op1, reads, writes):
        if op1 is None:
            return self.op(eng, lambda e: e.tensor_scalar(out_ap, a, s1, None, op0), reads, writes)
        return self.op(eng, lambda e: e.tensor_scalar(out_ap, a, s1, s2, op0, op1), reads, writes)

    def stt(self, out_ap, a, s, b, op0, op1, reads, writes):
        return self.op("dve", lambda e: e.scalar_tensor_tensor(out_ap, a, s, b, op0, op1), reads, writes)


class Phase:
    _uid = [0]

    def __init__(self, prog, name):
        self.prog, self.name = prog, name

    def __enter__(self):
        from contextlib import ExitStack
        self.es = ExitStack()
        self.es.__enter__()
        return self

    def sb(self, name, shape, dtype=F32):
        Phase._uid[0] += 1
        nm = f"{self.name}_{name}_{Phase._uid[0]}"
        t = self.es.enter_context(self.prog.nc.sbuf_tensor(nm, list(shape), dtype))
        return Buf(t, nm)

    def __exit__(self, *a):
        if a[0] is None:
            self.prog.k.barrier()
        return self.es.__exit__(*a)


class Prog:
    def __init__(self, cfg):
        self.cfg = cfg
        self.nc = bass.Bass("TRN2", target_bir_lowering=False)
        self.k = KB(self.nc)
        self.inputs = {}
        self.outputs = {}
        self.rr = 0

    def inp(self, name, shape, dtype=F32):
        b = Buf(self.nc.dram_tensor(name, list(shape), dtype, kind="ExternalInput"), name)
        self.inputs[name] = (tuple(shape), dtype)
        return b

    def scratch(self, name, shape, dtype=F32):
        kind = "ExternalOutput" if name in self.cfg.dump else "Internal"
        b = Buf(self.nc.dram_tensor(name, list(shape), dtype, kind=kind), name)
        if kind == "ExternalOutput":
            self.outputs[name] = (tuple(shape), dtype)
        return b

    def alt(self, engines=("act", "dve")):
        self.rr += 1
        return engines[self.rr % len(engines)]

    def build(self):
        cfg, k, nc = self.cfg, self.k, self.nc
        NB, L, C, S, NT = cfg.nb, cfg.L, cfg.C, cfg.S, cfg.NT
        NR = NB + 1
        DEP = cfg.depth

        xin = self.inp("xin", [NB * S, D])
        ctxin = self.inp("ctxin", [NB * C, D])
        ccT = self.inp("ccT", [128, KD * NR])
        w_mod = self.inp("w_mod", [DEP * 24 * 128, KD * 512])
        b_mod = self.inp("b_mod", [DEP, 6 * D])
        w_ext = self.inp("w_ext", [DEP * NT_EXT * 128, KD * 128])
        ident_f = self.inp("ident_f", [128, 128])
        self.out = Buf(nc.dram_tensor("out", [NB * S, D], F32, kind="ExternalOutput"), "out")
        self.outputs["out"] = ((NB * S, D), F32)

        xs = [self.scratch(f"xs{b}", [L, D]) for b in range(NB)]
        modv = self.scratch("modv", [DEP * NR, 6 * D])
        pT = self.scratch("pT", [N_EXT, L])

        identf = k.sb("identf", [128, 128], F32)
        identb = k.sb("identb", [128, 128], BF16)
        k.dma("sp", identf[:], ident_f.ap(), [ident_f], [identf])
        k.copy("dve", identb[:], identf[:], [identf], [identb])
        ps = [k.psum(f"ps{i}", [128, 512], F32) for i in range(8)]
        self.ps = ps

        for b in range(NB):
            k.dma("sp", xs[b][0:C, :], ctxin[b * C:(b + 1) * C, :], [ctxin], [xs[b]])
            for s0 in range(0, S, 512):
                s1 = min(S, s0 + 512)
                k.dma("pool" if (s0 // 512) % 2 else "act", xs[b][C + s0:C + s1, :], xin[b * S + s0:b * S + s1, :], [xin], [xs[b]])

        phm = Phase(self, "mod").__enter__()
        sc = phm.sb("mod_sc", [128, KD * NR], F32)
        k.dma("sp", sc[:], ccT.ap(), [ccT], [sc])
        k.act(sc[:], sc[:], AF.Silu, [sc], [sc])
        wm = [phm.sb(f"mod_w{i}", [128, KD * 512], F32) for i in range(2)]
        mo = [phm.sb(f"mod_o{i}", [NR, 512], F32) for i in range(2)]
        mb = [phm.sb(f"mod_b{i}", [NR, 512], F32) for i in range(2)]
        it = 0
        for l in range(DEP):
            for nb_ in range(24):
                w = wm[it % 2]
                o = mo[it % 2]
                bb = mb[it % 2]
                pst = ps[it % 2]
                r0 = (l * 24 + nb_) * 128
                k.dma("sp" if it % 2 == 0 else "act", w[:], w_mod[r0:r0 + 128, :], [w_mod], [w])
                k.dma("pool", bb[:], b_mod[l:l + 1, nb_ * 512:(nb_ + 1) * 512].partition_broadcast(NR),
                      [b_mod], [bb])
                for kk in range(KD):
                    k.mm(pst[0:NR, :], sc[:, kk * NR:(kk + 1) * NR], w[:, kk * 512:(kk + 1) * 512],
                         kk == 0, kk == KD - 1, [sc, w], [pst])
                k.tt("dve", o[:], pst[0:NR, :], bb[:], ALU.add, [pst, bb], [o])
                k.dma("pool", modv[l * NR:(l + 1) * NR, nb_ * 512:(nb_ + 1) * 512], o[:], [o], [modv])
                it += 1
        phm.__exit__(None, None, None)
        if cfg.stop_after == "mod":
            return self.finish()

        self.xin, self.ctxin, self.xs, self.modv, self.pT = xin, ctxin, xs, modv, pT
        self.identf, self.identb = identf, identb
        self.w_ext = w_ext
        self.NR = NR
        self.decl_rest()
        self.decl_rwkv()
        self.decl_moe()
        for l in range(DEP):
            for b in range(NB):
                self.phase_proj(l, b)
                if cfg.stop_after == "proj":
                    return self.finish()
                if "noattn" not in cfg.dump:
                    self.phase_diff(l, b)
                    self.phase_win(l, b)
                if cfg.stop_after == "attn":
                    return self.finish()
                self.phase_rwkv_feat(l, b)
                if cfg.stop_after == "rwf":
                    return self.finish()
                self.phase_rwkv_scan(l, b)
                if cfg.stop_after == "rws":
                    return self.finish()
                self.phase_merge(l, b)
                if cfg.stop_after == "mrg":
                    return self.finish()
                self.phase_out(l, b)
                if cfg.stop_after == "out":
                    return self.finish()
            if "cnt" in cfg.dump:
                k.dma("sp", self.cnt_out[l * 128:(l + 1) * 128, :], self.off[:], [self.off], [self.cnt_out])
            self.phase_experts(l)
            for b in range(NB):
                self.phase_combine(l, b)
        return self.finish()

    def phase(self, name):
        return Phase(self, name)

    def decl_rest(self):
        cfg, k, nc = self.cfg, self.k, self.nc
        DEP, L = cfg.depth, cfg.L
        self.ropec = self.inp("ropec", [128, L])
        self.ropes = self.inp("ropes", [128, L])
        self.diff_lam = self.inp("diff_lam", [DEP, 256])
        self.diff_subln = self.inp("diff_subln", [DEP, 128])
        self.win_sink = self.inp("win_sink", [DEP, 8])
        self.maskP_in = self.inp("maskP", [128, 128])
        self.maskN_in = self.inp("maskN", [128, 128])
        self.yT = self.scratch("yT", [D, L], BF16)
        self.ones_bf = k.sb("ones_bf", [128, 128], BF16)
        k.op("dve", lambda e: e.memset(self.ones_bf[:], 1.0), [], [self.ones_bf])
        self.ones_f = k.sb("ones_f", [128, 128], F32)
        k.op("dve", lambda e: e.memset(self.ones_f[:], 1.0), [], [self.ones_f])
        self.eps_ada = k.sb("eps_ada", [128, 1], F32)
        k.op("dve", lambda e: e.memset(self.eps_ada[:], ADA_EPS), [], [self.eps_ada])
        self.eps_ln = k.sb("eps_ln", [128, 1], F32)
        k.op("dve", lambda e: e.memset(self.eps_ln[:], LN_EPS), [], [self.eps_ln])
        self.eps_5 = self.eps_ln
        mtmp = k.sb("mtmp", [128, 128], F32)
        self.maskP = k.sb("maskP_sb", [128, 128], BF16)
        self.maskN = k.sb("maskN_sb", [128, 128], BF16)
        k.dma("sp", mtmp[:], self.maskP_in.ap(), [self.maskP_in], [mtmp])
        k.copy("dve", self.maskP[:], mtmp[:], [mtmp], [self.maskP])
        k.dma("sp", mtmp[:], self.maskN_in.ap(), [self.maskN_in], [mtmp])
        k.copy("dve", self.maskN[:], mtmp[:], [mtmp], [self.maskN])

    def load_bc(self, dst, l, r, j, add_one):
        k = self.k
        NR = self.NR
        k.dma("sp", dst[:], self.modv[l * NR + r:l * NR + r + 1, j * D:(j + 1) * D].partition_broadcast(128),
              [self.modv], [dst])
        if add_one:
            k.ts("pool", dst[:], dst[:], 1.0, None, ALU.add, None, [dst], [dst])

    def ln_stats(self, ph_tiles, x_tile, eps_tile):
        k = self.k
        stats, mv, rstd = ph_tiles
        for c4 in range(4):
            k.op("dve", lambda e, c4=c4: e.bn_stats(stats[:, c4, :], x_tile[:, c4 * 512:(c4 + 1) * 512]),
                 [x_tile], [stats])
        k.op("dve", lambda e: e.bn_aggr(mv[:], stats[:]), [stats], [mv])
        k.act(rstd[:], mv[:, 1:2], AF.Sqrt, [mv, eps_tile], [rstd], bias=eps_tile[:], scale=1.0)
        k.op("dve", lambda e: e.reciprocal(rstd[:], rstd[:]), [rstd], [rstd])

    def phase_proj(self, l, b):
        cfg, k, ps = self.cfg, self.k, self.ps
        NB, L, C, NT = cfg.nb, cfg.L, cfg.C, cfg.NT
        xs, pT, identb = self.xs, self.pT, self.identb
        with self.phase(f"proj{l}_{b}") as ph:
            uT = ph.sb("uT", [128, KD, L], BF16)
            xt = [ph.sb(f"xt{i}", [128, D], F32) for i in range(2)]
            ub = [ph.sb(f"ub{i}", [128, D], BF16) for i in range(2)]
            bc = {nm: ph.sb(nm, [128, D], F32) for nm in ("sc", "sh", "scc", "shc")}
            st3 = (ph.sb("stats", [128, 4, 6], F32), ph.sb("mv", [128, 2], F32), ph.sb("rstd", [128, 1], F32))
            wst = [ph.sb(f"wst{i}", [128, KD * 128], F32) for i in range(2)]
            wbf = [ph.sb(f"wbf{i}", [128, KD, 128], BF16) for i in range(3)]
            ot = [ph.sb(f"ot{i}", [128, L], F32) for i in range(2)]
            self.load_bc(bc["scc"], l, NB, 1, True)
            self.load_bc(bc["shc"], l, NB, 0, False)
            self.load_bc(bc["sc"], l, b, 1, True)
            self.load_bc(bc["sh"], l, b, 0, False)
            for t in range(NT):
                x_t = xt[t % 2]
                u_t = ub[t % 2]
                k.dma("sp" if t % 2 == 0 else "act", x_t[:], xs[b][t * 128:(t + 1) * 128, :], [xs[b]], [x_t])
                isctx = t * 128 < C
                sc_t, sh_t = (bc["scc"], bc["shc"]) if isctx else (bc["sc"], bc["sh"])
                self.ln_stats(st3, x_t, self.eps_ada)
                k.ts("dve", x_t[:], x_t[:], st3[1][:, 0:1], st3[2][:, 0:1], ALU.subtract, ALU.mult,
                     [x_t, st3[1], st3[2]], [x_t])
                k.tt("pool", x_t[:], x_t[:], sc_t[:], ALU.mult, [x_t, sc_t], [x_t])
                k.tt("dve", u_t[:], x_t[:], sh_t[:], ALU.add, [x_t, sh_t], [u_t])
                for g4 in range(4):
                    pst = ps[4 + (t * 4 + g4) % 4]
                    pv = pst.ap().bitcast(BF16)
                    for j in range(4):
                        kk = g4 * 4 + j
                        k.tr(pv[:, j * 128:(j + 1) * 128], u_t[:, kk * 128:(kk + 1) * 128], identb[:],
                             [u_t, identb], [pst])
                    k.copy(self.alt(), uT[:, g4 * 4:(g4 + 1) * 4, t * 128:(t + 1) * 128],
                           pv[:, 0:512].rearrange("p (j n) -> p j n", j=4), [pst], [uT])
            nblk = (L + 511) // 512
            for ct in range(NT_EXT):
                st_ = wst[ct % 2]
                wb = wbf[ct % 3]
                o_t = ot[ct % 2]
                r0 = (l * NT_EXT + ct) * 128
                k.dma("sp" if ct % 2 == 0 else "act", st_[:], self.w_ext[r0:r0 + 128, :], [self.w_ext], [st_])
                k.copy(self.alt(("dve", "pool")), wb[:], st_[:].rearrange("p (k n) -> p k n", k=KD), [st_], [wb])
                for tb in range(nblk):
                    t0 = tb * 512
                    n = min(512, L - t0)
                    pst = ps[(ct * nblk + tb) % 4]
                    for kk in range(KD):
                        k.mm(pst[:, 0:n], wb[:, kk, :], uT[:, kk, t0:t0 + n], kk == 0, kk == KD - 1,
                             [wb, uT], [pst])
                    k.copy(self.alt(), o_t[:, t0:t0 + n], pst[:, 0:n], [pst], [o_t])
                k.dma("pool", pT[ct * 128:(ct + 1) * 128, :], o_t[:], [o_t], [pT.sub(ct)])

    def rope_tile(self, ph, dst_buf, dst_bf, row0, prow0, tmp_a, tmp_b, rc, rs):
        k, pT, L = self.k, self.pT, self.cfg.L
        ct, pt_ = row0 // 128, prow0 // 128
        k.dma("sp", tmp_a[:], pT[row0:row0 + 128, :], [pT.sub(ct)], [tmp_a])
        k.dma("act", tmp_b[:], pT[prow0:prow0 + 128, :], [pT.sub(pt_)], [tmp_b])
        k.tt("pool", tmp_a[:], tmp_a[:], rc[:], ALU.mult, [tmp_a, rc], [tmp_a])
        k.tt("dve", tmp_b[:], tmp_b[:], rs[:], ALU.mult, [tmp_b, rs], [tmp_b])
        k.tt("dve", dst_bf, tmp_a[:], tmp_b[:], ALU.add, [tmp_a, tmp_b], [dst_buf])

    def to_tokmajor(self, ph, dst_tok, row0, tmp_a, tmp_bf):
        k, pT, ps, NT = self.k, self.pT, self.ps, self.cfg.NT
        ct = row0 // 128
        k.dma("sp", tmp_a[:], pT[row0:row0 + 128, :], [pT.sub(ct)], [tmp_a])
        k.copy("pool", tmp_bf[:], tmp_a[:], [tmp_a], [tmp_bf])
        for t0 in range(0, NT, 4):
            nt = min(4, NT - t0)
            pst = ps[4 + (t0 // 4) % 4]
            pv = pst.ap().bitcast(BF16)
            for j in range(nt):
                t = t0 + j
                k.tr(pv[:, j * 128:(j + 1) * 128], tmp_bf[:, t * 128:(t + 1) * 128], self.identb[:],
                     [tmp_bf, self.identb], [pst])
            k.copy(self.alt(), dst_tok[:, t0:t0 + nt, :],
                   pv[:, 0:nt * 128].rearrange("p (j n) -> p j n", j=nt), [pst], [dst_tok])

    def phase_diff(self, l, b):
        cfg, k, ps = self.cfg, self.k, self.ps
        L, C, S, NT = cfg.L, cfg.C, cfg.S, cfg.NT
        lam_init = 0.8 - 0.6 * math.exp(-0.3 * l)
        scale = 64 ** -0.5
        sec = EXT_SEC
        with self.phase(f"diff{l}_{b}") as ph:
            rc = ph.sb("rc", [128, L], F32)
            rs = ph.sb("rs", [128, L], F32)
            k.dma("sp", rc[:], self.ropec.ap(), [self.ropec], [rc])
            k.dma("act", rs[:], self.ropes.ap(), [self.ropes], [rs])
            ta = ph.sb("ta", [128, L], F32)
            tb_ = ph.sb("tb", [128, L], F32)
            tbf = ph.sb("tbf", [128, L], BF16)
            qr = ph.sb("qr", [128, L], BF16)
            kr = ph.sb("kr", [128, L], BF16)
            vtok = ph.sb("vtok", [128, NT, 128], BF16)
            pt = [ph.sb(f"pt{i}", [128, 512], BF16) for i in range(2)]
            rl = ph.sb("rl", [128, 512], F32)
            o0 = ph.sb("o0", [128, 512], F32)
            o1 = ph.sb("o1", [128, 512], F32)
            sq = ph.sb("sq", [128, 512], F32)
            yo = ph.sb("yo", [128, 512], BF16)
            dl = ph.sb("dl", [128, 256], F32)
            pr = ph.sb("pr", [128, 128], F32)
            e2 = ph.sb("e2", [128, 2], F32)
            nlam = ph.sb("nlam", [128, 1], F32)
            gsc = ph.sb("gsc", [128, 1], F32)
            k.dma("sp", dl[:], self.diff_lam[l:l + 1, :].partition_broadcast(128), [self.diff_lam], [dl])
            k.tt("dve", pr[:].rearrange("p (a d) -> p a d", a=2),
                 dl[:].rearrange("p (a t d) -> p a t d", a=2, t=2)[:, :, 0, :],
                 dl[:].rearrange("p (a t d) -> p a t d", a=2, t=2)[:, :, 1, :], ALU.mult, [dl], [pr])
            k.op("dve", lambda e: e.tensor_reduce(e2[:], pr[:].rearrange("p (a d) -> p a d", a=2), AX.X, ALU.add),
                 [pr], [e2])
            k.act(e2[:], e2[:], AF.Exp, [e2], [e2])
            k.tt("dve", nlam[:], e2[:, 1:2], e2[:, 0:1], ALU.subtract, [e2], [nlam])
            k.ts("dve", nlam[:], nlam[:], -lam_init, None, ALU.add, None, [nlam], [nlam])
            k.dma("sp", gsc[:], self.diff_subln[l:l + 1, :].rearrange("o (p q) -> (o p) q", q=1), [self.diff_subln], [gsc])
            k.ts("dve", gsc[:], gsc[:], 1.0 - lam_init, None, ALU.mult, None, [gsc], [gsc])
            qblocks = [(0, C, list(range(C // 128)))]
            q0 = C
            while q0 < L:
                nq = min(512, L - q0)
                qblocks.append((q0, nq, list(range(NT))))
                q0 += nq
            for h in range(6):
                self.rope_tile(ph, qr, qr[:], sec["dq"][0] + h * 128, sec["dqp"][0] + h * 128, ta, tb_, rc, rs)
                self.rope_tile(ph, kr, kr[:], sec["dk"][0] + h * 128, sec["dkp"][0] + h * 128, ta, tb_, rc, rs)
                self.to_tokmajor(ph, vtok, sec["dv"][0] + h * 128, ta, tbf)
                for (q0, nq, kts) in qblocks:
                    for m in range(2):
                        pso, psl = ps[m], ps[2 + m]
                        for i, kt in enumerate(kts):
                            pss = ps[6 + (i % 2)]
                            p_t = pt[i % 2]
                            k.mm(pss[:, 0:nq], kr[m * 64:(m + 1) * 64, kt * 128:(kt + 1) * 128],
                                 qr[m * 64:(m + 1) * 64, q0:q0 + nq], True, True, [kr, qr], [pss])
                            k.act(p_t[:, 0:nq], pss[:, 0:nq], AF.Exp, [pss], [p_t], scale=scale)
                            k.mm(pso[:, 0:nq], vtok[:, kt, :], p_t[:, 0:nq], i == 0, i == len(kts) - 1,
                                 [vtok, p_t], [pso])
                            k.mm(psl[:, 0:nq], self.ones_bf[:], p_t[:, 0:nq], i == 0, i == len(kts) - 1,
                                 [self.ones_bf, p_t], [psl])
                    k.op("dve", lambda e: e.reciprocal(rl[:, 0:nq], ps[2][:, 0:nq]), [ps[2]], [rl])
                    k.tt("dve", o0[:, 0:nq], ps[0][:, 0:nq], rl[:, 0:nq], ALU.mult, [ps[0], rl], [o0])
                    k.op("dve", lambda e: e.reciprocal(rl[:, 0:nq], ps[3][:, 0:nq]), [ps[3]], [rl])
                    k.tt("dve", o1[:, 0:nq], ps[1][:, 0:nq], rl[:, 0:nq], ALU.mult, [ps[1], rl], [o1])
                    k.stt(o0[:, 0:nq], o1[:, 0:nq], nlam[:, 0:1], o0[:, 0:nq], ALU.mult, ALU.add, [o1, nlam, o0], [o0])
                    k.tt("pool", sq[:, 0:nq], o0[:, 0:nq], o0[:, 0:nq], ALU.mult, [o0], [sq])
                    k.mm(ps[6][:, 0:nq], self.ones_f[:], sq[:, 0:nq], True, True, [self.ones_f, sq], [ps[6]])
                    k.act(rl[:, 0:nq], ps[6][:, 0:nq], AF.Sqrt, [ps[6], self.eps_5], [rl], bias=self.eps_5[:], scale=1.0 / 128)
                    k.op("dve", lambda e: e.reciprocal(rl[:, 0:nq], rl[:, 0:nq]), [rl], [rl])
                    k.tt("dve", o0[:, 0:nq], o0[:, 0:nq], rl[:, 0:nq], ALU.mult, [o0, rl], [o0])
                    k.ts("dve", yo[:, 0:nq], o0[:, 0:nq], gsc[:, 0:1], None, ALU.mult, None, [o0, gsc], [yo])
                    k.dma("pool", self.yT[768 + h * 128:768 + (h + 1) * 128, q0:q0 + nq], yo[:, 0:nq], [yo],
                          [self.yT.sub(("b", h, q0))])

    def _mark_write(self, buf, srcs):
        k = self.k
        buf.last_w = ("dve", k.cnt["dve"])
        buf.readers = {}

    def phase_win(self, l, b):
        cfg, k, ps = self.cfg, self.k, self.ps
        L, C, S, NT = cfg.L, cfg.C, cfg.S, cfg.NT
        scale = 64 ** -0.5
        sec = EXT_SEC
        NCT = C // 128
        with self.phase(f"win{l}_{b}") as ph:
            rc = ph.sb("rc", [128, L], F32)
            rs = ph.sb("rs", [128, L], F32)
            k.dma("sp", rc[:], self.ropec.ap(), [self.ropec], [rc])
            k.dma("act", rs[:], self.ropes.ap(), [self.ropes], [rs])
            ta = ph.sb("ta", [128, L], F32)
            tb_ = ph.sb("tb", [128, L], F32)
            tbf = ph.sb("tbf", [128, L], BF16)
            wqr = ph.sb("wqr", [128, 4, L], BF16)
            wkr = ph.sb("wkr", [128, L], BF16)
            wvtok = ph.sb("wvtok", [128, NT, 128], BF16)
            pt = [ph.sb(f"pt{i}", [128, 4, 128], BF16) for i in range(2)]
            den = ph.sb("den", [64, 4, 128], F32)
            yo = ph.sb("yo", [64, 4, 128], BF16)
            es = ph.sb("es", [64, 8], F32)
            k.dma("sp", es[:], self.win_sink[l:l + 1, :].partition_broadcast(64), [self.win_sink], [es])
            k.act(es[:], es[:], AF.Exp, [es], [es])
            for g in range(4):
                self.rope_tile(ph, wqr, wqr[:, g, :], sec["wq"][0] + g * 128, sec["wqp"][0] + g * 128, ta, tb_, rc, rs)
            self.rope_tile(ph, wkr, wkr[:], sec["wk"][0], sec["wkp"][0], ta, tb_, rc, rs)
            self.to_tokmajor(ph, wvtok, sec["wv"][0], ta, tbf)
            for hkv in range(2):
                pb = slice(hkv * 64, (hkv + 1) * 64)
                for qi in range(NT):
                    if qi < NCT:
                        kts = [(kt, None) for kt in range(NCT)]
                    else:
                        j = qi - NCT
                        kts = [(kt, None) for kt in range(NCT)]
                        if j - 1 >= 0:
                            kts.append((qi - 1, self.maskP))
                        kts.append((qi, None))
                        if j + 1 < S // 128:
                            kts.append((qi + 1, self.maskN))
                    pso, psl = ps[0 + (qi % 2)], ps[2 + (qi % 2)]
                    for i, (kt, msk) in enumerate(kts):
                        pss = ps[6 + (i % 2)]
                        p_t = pt[i % 2]
                        k.mm(pss[:, :].rearrange("p (g n) -> p g n", g=4), wkr[pb, kt * 128:(kt + 1) * 128],
                             wqr[pb, :, qi * 128:(qi + 1) * 128], True, True, [wkr, wqr], [pss])
                        k.act(p_t[:].rearrange("p g n -> p (g n)"), pss[:, :], AF.Exp, [pss], [p_t], scale=scale)
                        if msk is not None:
                            k.tt("pool", p_t[:], p_t[:], msk[:].rearrange("p (o n) -> p o n", o=1).broadcast_to([128, 4, 128]),
                                 ALU.mult, [p_t, msk], [p_t])
                        k.mm(pso[0:64, :], wvtok[:, kt, pb], p_t[:].rearrange("p g n -> p (g n)"), i == 0,
                             i == len(kts) - 1, [wvtok, p_t], [pso])
                        k.mm(psl[0:64, :], self.ones_bf[:, 0:64], p_t[:].rearrange("p g n -> p (g n)"), i == 0,
                             i == len(kts) - 1, [self.ones_bf, p_t], [psl])
                    k.tt("dve", den[:], psl[0:64, :].rearrange("p (g n) -> p g n", g=4),
                         es[:, hkv * 4:(hkv + 1) * 4].rearrange("p (g o) -> p g o", o=1).broadcast_to([64, 4, 128]),
                         ALU.add, [psl, es], [den])
                    k.op("dve", lambda e: e.reciprocal(den[:], den[:]), [den], [den])
                    k.tt("dve", yo[:], pso[0:64, :].rearrange("p (g n) -> p g n", g=4), den[:], ALU.mult,
                         [pso, den], [yo])
                    r0 = 1536 + hkv * 256
                    k.dma("pool", self.yT[r0:r0 + 256, qi * 128:(qi + 1) * 128].rearrange("(g d) n -> d g n", g=4),
                          yo[:], [yo], [self.yT.sub(("c", hkv, qi))])

    def decl_rwkv(self):
        cfg, k = self.cfg, self.k
        DEP, L = cfg.depth, cfg.L
        self.rwvec = self.inp("rwvec", [DEP * 128, 96])
        self.w_up = self.inp("w_up", [DEP * 128, 768])
        self.a_up = self.inp("a_up", [DEP * 128, 768])
        self.g_up = self.inp("g_up", [DEP * 128, 768])
        self.maskA_in = self.inp("maskA", [2 * 128, 512])
        self.maskB_in = self.inp("maskB", [2 * 128, 128])
        self.ident2_in = self.inp("ident2", [128, 128])
        self.bones_in = self.inp("bones", [128, 128])
        self.resetm_in = self.inp("resetm", [128, L])
        for nm in ("fr", "fv", "fg", "fkk", "fbv", "flw0", "flw1", "fkd0", "fkd1", "fbd0", "fbd1"):
            setattr(self, nm, self.scratch(nm, [768, L]))
        self.bones = k.sb("bones_sb", [128, 128], F32)
        k.dma("sp", self.bones[:], self.bones_in.ap(), [self.bones_in], [self.bones])
        self.eps_gn = k.sb("eps_gn", [128, 1], F32)
        k.op("dve", lambda e: e.memset(self.eps_gn[:], 64e-5), [], [self.eps_gn])

    def phase_rwkv_feat(self, l, b):
        cfg, k, ps, pT = self.cfg, self.k, self.ps, self.pT
        L, C, S = cfg.L, cfg.C, cfg.S
        segs = [(0, C), (C, L)]
        nblk = (L + 511) // 512
        with self.phase(f"rwf{l}_{b}") as ph:
            vec = ph.sb("vec", [128, 96], F32)
            k.dma("sp", vec[:], self.rwvec[l * 128:(l + 1) * 128, :], [self.rwvec], [vec])
            cm = ph.sb("cm", [128, 21], F32)
            k.tt("dve", cm[:], vec[:, 0:21], vec[:, 21:42], ALU.add, [vec], [cm])
            k.ts("dve", cm[:], cm[:], -1.0, 1.0, ALU.mult, ALU.add, [cm], [cm])
            omka = ph.sb("omka", [128, 6], F32)
            k.ts("dve", omka[:], vec[:, 72:78], -1.0, 1.0, ALU.mult, ALU.add, [vec], [omka])
            wup = ph.sb("wup", [128, 768], F32)
            aup = ph.sb("aup", [128, 768], F32)
            gup = ph.sb("gup", [128, 768], F32)
            k.dma("sp", wup[:], self.w_up[l * 128:(l + 1) * 128, :], [self.w_up], [wup])
            k.dma("act", aup[:], self.a_up[l * 128:(l + 1) * 128, :], [self.a_up], [aup])
            k.dma("sp", gup[:], self.g_up[l * 128:(l + 1) * 128, :], [self.g_up], [gup])
            pin = [ph.sb(f"pin{i}", [128, L], F32) for i in range(2)]
            twd = ph.sb("twd", [128, L], F32)
            ads = ph.sb("ads", [128, L], F32)
            sgd = ph.sb("sgd", [128, L], F32)
            rS = ph.sb("rS", [128, L], F32)
            vS = ph.sb("vS", [128, L], F32)
            kS = ph.sb("kS", [128, L], F32)
            kk_ = ph.sb("kk", [128, L], F32)
            t1 = ph.sb("t1", [128, L], F32)
            t2 = ph.sb("t2", [128, L], F32)
            aT = [ph.sb(f"aT{d}", [128, L], F32) for d in range(2)]
            kdT = [ph.sb(f"kdT{d}", [128, L], F32) for d in range(2)]
            self._pin_i = 0

            def shifted(tile_idx, dst):
                p_in = pin[self._pin_i % 2]
                self._pin_i += 1
                k.dma("sp" if self._pin_i % 2 else "act", p_in[:], pT[tile_idx * 128:(tile_idx + 1) * 128, :],
                      [pT.sub(tile_idx)], [p_in])
                k.ts("dve", dst[:], p_in[:], cm[:, tile_idx:tile_idx + 1], None, ALU.mult, None, [p_in, cm], [dst])
                for (a, e) in segs:
                    k.stt(dst[:, a + 1:e], p_in[:, a:e - 1], vec[:, tile_idx:tile_idx + 1], dst[:, a + 1:e],
                          ALU.mult, ALU.add, [p_in, vec, dst], [dst])
                    k.stt(dst[:, a:e - 1], p_in[:, a + 1:e], vec[:, 21 + tile_idx:22 + tile_idx], dst[:, a:e - 1],
                          ALU.mult, ALU.add, [p_in, vec, dst], [dst])

            shifted(18, twd)
            k.act(twd[:], twd[:], AF.Tanh, [twd], [twd])
            shifted(19, ads)
            shifted(20, sgd)
            k.act(sgd[:], sgd[:], AF.Sigmoid, [sgd], [sgd])
            flw = [self.flw0, self.flw1]
            fkd = [self.fkd0, self.fkd1]
            fbd = [self.fbd0, self.fbd1]
            NEG_EH = -math.exp(-0.5)
            for c in range(6):
                cs = slice(c * 128, (c + 1) * 128)
                for d in range(2):
                    pb = slice(d * 64, (d + 1) * 64)
                    for tb in range(nblk):
                        t0 = tb * 512
                        n = min(512, L - t0)
                        pst = ps[(tb) % 4]
                        k.mm(pst[:, 0:n], wup[pb, cs], twd[pb, t0:t0 + n], True, True, [wup, twd], [pst])
                        k.act(t1[:, t0:t0 + n], pst[:, 0:n], AF.Sigmoid, [pst, vec], [t1],
                              bias=vec[:, 42 + d * 6 + c:43 + d * 6 + c], scale=1.0)
                        pst2 = ps[4 + (tb) % 4]
                        k.mm(pst2[:, 0:n], aup[pb, cs], ads[pb, t0:t0 + n], True, True, [aup, ads], [pst2])
                        k.act(aT[d][:, t0:t0 + n], pst2[:, 0:n], AF.Sigmoid, [pst2, vec], [aT[d]],
                              bias=vec[:, 54 + d * 6 + c:55 + d * 6 + c], scale=1.0)
                    k.ts("pool", t1[:], t1[:], NEG_EH, None, ALU.mult, None, [t1], [t1])
                    k.dma("pool", flw[d][cs, :], t1[:], [t1], [flw[d].sub(c)])
                for tb in range(nblk):
                    t0 = tb * 512
                    n = min(512, L - t0)
                    pst = ps[tb % 4]
                    k.mm(pst[:, 0:n], gup[:, cs], sgd[:, t0:t0 + n], True, True, [gup, sgd], [pst])
                    k.copy("act", t2[:, t0:t0 + n], pst[:, 0:n], [pst], [t2])
                k.dma("pool", self.fg[cs, :], t2[:], [t2], [self.fg.sub(c)])
                shifted(c, rS)
                k.dma("pool", self.fr[cs, :], rS[:], [rS], [self.fr.sub(c)])
                shifted(12 + c, vS)
                k.dma("pool", self.fv[cs, :], vS[:], [vS], [self.fv.sub(c)])
                shifted(6 + c, kS)
                k.ts("dve", kk_[:], kS[:], vec[:, 66 + c:67 + c], None, ALU.mult, None, [kS, vec], [kk_])
                k.tt("pool", t1[:], kk_[:], kk_[:], ALU.mult, [kk_], [t1])
                for tb in range(nblk):
                    t0 = tb * 512
                    n = min(512, L - t0)
                    pst = ps[tb % 4]
                    k.mm(pst[:, 0:n], self.bones[:], t1[:, t0:t0 + n], True, True, [self.bones, t1], [pst])
                    k.act(t2[:, t0:t0 + n], pst[:, 0:n], AF.Sqrt, [pst], [t2])
                k.ts("dve", t2[:], t2[:], 1e-12, None, ALU.max, None, [t2], [t2])
                k.op("dve", lambda e: e.reciprocal(t2[:], t2[:]), [t2], [t2])
                k.tt("dve", kk_[:], kk_[:], t2[:], ALU.mult, [kk_, t2], [kk_])
                k.dma("pool", self.fkk[cs, :], kk_[:], [kk_], [self.fkk.sub(c)])
                for d in range(2):
                    k.ts("dve", t1[:], aT[d][:], vec[:, 72 + c:73 + c], omka[:, c:c + 1], ALU.mult, ALU.add,
                         [aT[d], vec, omka], [t1])
                    k.tt("dve", kdT[d][:], t1[:], kS[:], ALU.mult, [t1, kS], [kdT[d]])
                    k.dma("pool", fkd[d][cs, :], kdT[d][:], [kdT[d]], [fkd[d].sub(c)])
                    k.tt("pool", t2[:], kk_[:], aT[d][:], ALU.mult, [kk_, aT[d]], [t2])
                    k.dma("pool", fbd[d][cs, :], t2[:], [t2], [fbd[d].sub(c)])
                k.tt("dve", t1[:], kdT[0][:], kdT[1][:], ALU.add, [kdT[0], kdT[1]], [t1])
                k.tt("dve", t1[:], t1[:], rS[:], ALU.mult, [t1, rS], [t1])
                k.ts("dve", t1[:], t1[:], vec[:, 78 + c:79 + c], None, ALU.mult, None, [t1, vec], [t1])
                for tb in range(nblk):
                    t0 = tb * 512
                    n = min(512, L - t0)
                    pst = ps[tb % 4]
                    k.mm(pst[:, 0:n], self.bones[:], t1[:, t0:t0 + n], True, True, [self.bones, t1], [pst])
                    k.tt("dve", t2[:, t0:t0 + n], pst[:, 0:n], vS[:, t0:t0 + n], ALU.mult, [pst, vS], [t2])
                k.dma("pool", self.fbv[cs, :], t2[:], [t2], [self.fbv.sub(c)])

    def phase_rwkv_scan(self, l, b):
        cfg, k, ps = self.cfg, self.k, self.ps
        L, C, S, NT = cfg.L, cfg.C, cfg.S, cfg.NT
        NCH = L // 64
        NCC = C // 64
        nblk = (L + 511) // 512
        flw = [self.flw0, self.flw1]
        fkd = [self.fkd0, self.fkd1]
        fbd = [self.fbd0, self.fbd1]
        with self.phase(f"rws{l}_{b}") as ph:
            vec = ph.sb("vec", [128, 96], F32)
            k.dma("sp", vec[:], self.rwvec[l * 128:(l + 1) * 128, :], [self.rwvec], [vec])
            maskA = [ph.sb(f"maskA{d}", [64, 512], F32) for d in range(2)]
            maskB = [ph.sb(f"maskB{d}", [64, 128], F32) for d in range(2)]
            for d in range(2):
                k.dma("sp", maskA[d][:], self.maskA_in[d * 128:d * 128 + 64, :], [self.maskA_in], [maskA[d]])
                k.dma("act", maskB[d][:], self.maskB_in[d * 128:d * 128 + 64, :], [self.maskB_in], [maskB[d]])
            ident2 = ph.sb("ident2", [64, 128], F32)
            k.dma("sp", ident2[:], self.ident2_in[0:64, :], [self.ident2_in], [ident2])
            resetm = ph.sb("resetm", [128, L], F32)
            k.dma("sp", resetm[:], self.resetm_in.ap(), [self.resetm_in], [resetm])
            big1 = ph.sb("big1", [128, 2 * L], F32)
            big2 = ph.sb("big2", [128, 2 * L], F32)
            big3 = ph.sb("big3", [128, L], F32)
            katBD_t = ph.sb("katBD", [128, 2 * L], BF16)
            btBD_t = ph.sb("btBD", [128, 2 * L], BF16)
            rtBD_t = ph.sb("rtBD", [128, 2 * L], BF16)
            E1 = ph.sb("E1", [128, L], F32)
            rt32 = ph.sb("rt32", [128, L], F32)
            kat32 = ph.sb("kat32", [128, L], F32)
            kt32 = ph.sb("kt32", [128, L], F32)
            bt32 = ph.sb("bt32", [128, L], F32)
            rt = ph.sb("rt", [128, L], BF16)
            kat = ph.sb("kat", [128, L], BF16)
            kt_ = ph.sb("kt", [128, L], BF16)
            bt = ph.sb("bt", [128, L], BF16)
            Ktok = ph.sb("Ktok", [64, NCH, 128], BF16)
            Btok = ph.sb("Btok", [64, NCH, 128], BF16)
            Vtok = ph.sb("Vtok", [64, NCH, 128], BF16)
            yacc = ph.sb("yacc", [128, L], F32)
            Asb = ph.sb("Asb", [64, 8, 64], BF16)
            Bsb = ph.sb("Bsb", [64, 2, 64], BF16)
            Psb = ph.sb("Psb", [64, 4, 64], BF16)
            Rsb = ph.sb("Rsb", [64, 2, 64], BF16)
            Asb2 = ph.sb("Asb2", [64, 8, 64], BF16)
            Bsb2 = ph.sb("Bsb2", [64, 2, 64], BF16)
            Rsb2 = ph.sb("Rsb2", [64, 2, 64], BF16)
            Wsb = ph.sb("Wsb", [64, 2, 64], BF16)
            Usb = ph.sb("Usb", [64, 2, 64], BF16)
            H = ph.sb("H", [128, 64], F32)
            Hbd = ph.sb("Hbd", [128, 128], BF16)
            Hbd32 = ph.sb("Hbd32", [128, 128], F32)
            Htmp = ph.sb("Htmp", [128, 128], F32)
            yo = ph.sb("yo", [128, L], BF16)
            lw, cl = big1[:, 0:L], big1[:, L:2 * L]
            E2, t1 = big2[:, 0:L], big2[:, L:2 * L]
            vv = big3[:, 0:L]

            def tok_major(dst, src_buf, src_ap, neg=False, bf=True):
                for c0 in range(0, NCH, 4):
                    n4 = min(4, NCH - c0)
                    pst = ps[4 + (c0 // 4) % 2]
                    pv = pst.ap().bitcast(BF16) if bf else pst.ap()
                    idn = self.identb if bf else self.identf
                    for j in range(n4):
                        ch = c0 + j
                        k.tr(pv[0:64, j * 128:(j + 1) * 128], src_ap[:, ch * 64:(ch + 1) * 64], idn[:],
                             [src_buf, idn], [pst])
                    if neg:
                        k.ts("dve", dst[:, c0:c0 + n4, :], pv[0:64, 0:n4 * 128].rearrange("p (j n) -> p j n", j=n4),
                             -1.0, None, ALU.mult, None, [pst], [dst])
                    else:
                        k.copy(self.alt(), dst[:, c0:c0 + n4, :],
                               pv[0:64, 0:n4 * 128].rearrange("p (j n) -> p j n", j=n4), [pst], [dst])

            def make_bd(dst_big, src):
                d4 = dst_big[:].rearrange("p (c h n) -> p c h n", h=2, n=64)
                s3 = src[:].rearrange("p (c n) -> p c n", n=64)
                k.op("pool", lambda e: e.memset(dst_big[:], 0.0), [], [dst_big])
                k.copy("dve", d4[0:64, :, 0, :], s3[0:64], [src], [dst_big])
                k.copy("act", d4[64:128, :, 1, :], s3[64:128], [src], [dst_big])

            for c in range(6):
                cs = slice(c * 128, (c + 1) * 128)
                k.dma("sp", vv, self.fv[cs, :], [self.fv.sub(c)], [big3])
                tok_major(Vtok, big3, vv, bf=False)
                for d in range(2):
                    k.dma("sp", lw, flw[d][cs, :], [flw[d].sub(c)], [big1])
                    k.dma("act", rt32[:], self.fr[cs, :], [self.fr.sub(c)], [rt32])
                    k.dma("sp", kat32[:], self.fkk[cs, :], [self.fkk.sub(c)], [kat32])
                    k.dma("act", kt32[:], fkd[d][cs, :], [fkd[d].sub(c)], [kt32])
                    k.dma("sp", bt32[:], fbd[d][cs, :], [fbd[d].sub(c)], [bt32])
                    k.op("dve", lambda e: e.tensor_tensor_scan(cl, resetm[:], lw, 0.0, ALU.mult, ALU.add),
                         [resetm, big1], [big1])
                    if d == 1:
                        cl3 = cl.rearrange("p (c n) -> p c n", n=64)
                        k.tt("dve", t1.rearrange("p (c n) -> p c n", n=64), cl3[:, :, 63:64].broadcast_to([128, NCH, 64]),
                             cl3, ALU.subtract, [big1], [big2])
                        k.tt("dve", cl, t1, lw, ALU.add, [big2, big1], [big1])
                    k.act(E1[:], cl, AF.Exp, [big1], [E1])
                    k.act(E2, cl, AF.Exp, [big1], [big2], scale=-1.0)
                    k.tt("dve", t1, cl, lw, ALU.subtract, [big1], [big2])
                    k.act(t1, t1, AF.Exp, [big2], [big2])
                    k.tt("dve", rt[:], rt32[:], E1[:], ALU.mult, [rt32, E1], [rt])
                    k.tt("pool", kat[:], kat32[:], t1, ALU.mult, [kat32, big2], [kat])
                    k.tt("dve", kt_[:], kt32[:], E2, ALU.mult, [kt32, big2], [kt_])
                    k.tt("pool", bt[:], bt32[:], E2, ALU.mult, [bt32, big2], [bt])
                    tok_major(Ktok, kt_, kt_[:])
                    tok_major(Btok, bt, bt[:], neg=True)
                    make_bd(katBD_t, kat)
                    make_bd(btBD_t, bt)
                    make_bd(rtBD_t, rt)
                    katBD = katBD_t[:].rearrange("p (c m) -> p c m", m=128)
                    btBD = btBD_t[:].rearrange("p (c m) -> p c m", m=128)
                    rtBD = rtBD_t[:].rearrange("p (c m) -> p c m", m=128)
                    k.op("dve", lambda e: e.memset(H[:], 0.0), [], [H])
                    k.op("dve", lambda e: e.memset(Hbd[:], 0.0), [], [Hbd])
                    k.op("dve", lambda e: e.memset(Hbd32[:], 0.0), [], [Hbd32])
                    if d == 0:
                        order = list(range(NCH))
                    else:
                        order = list(range(NCC - 1, -1, -1)) + list(range(NCH - 1, NCC - 1, -1))
                    psA, psB, psP, psR, psW, psY, psH = ps[0], ps[1], ps[2], ps[3], ps[6], ps[7], ps[5]
                    flat = lambda t: t[:].rearrange("p a n -> p (a n)")

                    def pre_gen(ch, A_, B_, R_):
                        cc = slice(ch * 64, (ch + 1) * 64)
                        k.mm(psA[0:64, 0:128], bt[:, cc], katBD[:, ch, :], True, True, [bt, katBD_t], [psA])
                        k.mm(psA[0:64, 128:256], kat[:, cc], btBD[:, ch, :], True, True, [kat, btBD_t], [psA])
                        k.mm(psA[0:64, 256:384], kt_[:, cc], katBD[:, ch, :], True, True, [kt_, katBD_t], [psA])
                        k.mm(psA[0:64, 384:512], kt_[:, cc], rtBD[:, ch, :], True, True, [kt_, rtBD_t], [psA])
                        k.mm(psB[0:64, 0:128], bt[:, cc], rtBD[:, ch, :], True, True, [bt, rtBD_t], [psB])
                        yield
                        k.tt("dve", flat(A_), psA[0:64, :], maskA[d][:], ALU.mult, [psA, maskA[d]], [A_])
                        k.tt("dve", flat(B_), psB[0:64, 0:128], maskB[d][:], ALU.mult, [psB, maskB[d]], [B_])
                        k.tt("dve", flat(R_), ident2[:], A_[:, 0:2, :].rearrange("p a n -> p (a n)"), ALU.subtract, [ident2, A_], [R_])
                        yield
                        Pc, PTc = (A_, 0), (A_, 2)
                        for lev in range(5):
                            last = lev == 4
                            for hh in range(2):
                                Pm = Pc[0][:, Pc[1] + hh, :]
                                PTm = PTc[0][:, PTc[1] + hh, :]
                                if not last:
                                    k.mm(psP[0:64, hh * 64:(hh + 1) * 64], PTm, Pm, True, True, [Pc[0]], [psP])
                                k.mm(psP[0:64, (2 + hh) * 64:(3 + hh) * 64], Pm, PTm, True, True, [Pc[0]], [psP])
                            yield
                            if not last:
                                k.copy("act", flat(Psb), psP[0:64, 0:256], [psP], [Psb])
                            else:
                                k.copy("act", Psb[:, 2:4, :].rearrange("p a n -> p (a n)"), psP[0:64, 128:256], [psP], [Psb])
                            yield
                            Pc, PTc = (Psb, 0), (Psb, 2)
                            for hh in range(2):
                                k.mm(psR[0:64, hh * 64:(hh + 1) * 64], Psb[:, 2 + hh, :], R_[:, hh, :], True, True, [Psb, R_], [psR])
                            yield
                            k.tt("dve", flat(R_), flat(R_), psR[0:64, 0:128], ALU.add, [R_, psR], [R_])
                            yield

                    def ser_gen(ch, A_, B_, R_):
                        cc = slice(ch * 64, (ch + 1) * 64)
                        k.mm(psW[0:64, 0:128], kat[:, cc], Hbd[:], True, False, [kat, Hbd], [psW])
                        for hh in range(2):
                            hp = slice(hh * 64, (hh + 1) * 64)
                            k.mm(psW[0:64, hh * 64:(hh + 1) * 64], A_[:, 4 + hh, :], Vtok[:, ch, hp], False, hh == 1, [A_, Vtok], [psW])
                        yield
                        k.copy("act", flat(Wsb), psW[0:64, 0:128], [psW], [Wsb])
                        yield
                        for hh in range(2):
                            k.mm(psW[0:64, (2 + hh) * 64:(3 + hh) * 64], R_[:, hh, :], Wsb[:, hh, :], True, True, [R_, Wsb], [psW])
                        yield
                        k.copy("act", flat(Usb), psW[0:64, 128:256], [psW], [Usb])
                        yield
                        ycol = slice((ch % 4) * 128, (ch % 4 + 1) * 128)
                        k.mm(psY[:, ycol], Hbd[:], rtBD[:, ch, :], True, False, [Hbd, rtBD_t], [psY])
                        k.mm(psY[:, ycol], Vtok[:, ch, :], A_[:, 6:8, :].rearrange("p a n -> p (a n)"), False, False, [Vtok, A_], [psY])
                        k.mm(psY[:, ycol], flat(Usb), flat(B_), False, True, [Usb, B_], [psY])
                        k.mm(psH[:, 0:128], Ktok[:, ch, :], Vtok[:, ch, :], True, False, [Ktok, Vtok], [psH])
                        k.mm(psH[:, 0:128], Btok[:, ch, :], flat(Usb), False, True, [Btok, Usb], [psH])
                        yield
                        gcol = ch * 64 + (63 if d == 0 else 0)
                        k.tt("dve", Htmp[:], Hbd32[:], psH[:, 0:128], ALU.add, [Hbd32, psH], [Htmp])
                        k.stt(Hbd32[:], Htmp[:], E1[:, gcol:gcol + 1], self.bones[:], ALU.mult, ALU.mult, [Htmp, E1, self.bones], [Hbd32])
                        k.copy("pool", Hbd[:], Hbd32[:], [Hbd32], [Hbd])
                        for hh in range(2):
                            hp = slice(hh * 64, (hh + 1) * 64)
                            ysrc = psY[hp, (ch % 4) * 128 + hh * 64:(ch % 4) * 128 + (hh + 1) * 64]
                            if d == 0:
                                k.copy("act", yacc[hp, cc], ysrc, [psY], [yacc])
                            else:
                                k.tt("pool" if False else "dve", yacc[hp, cc], yacc[hp, cc], ysrc, ALU.add, [yacc, psY], [yacc])
                        yield

                    bufs = [(Asb, Bsb, Rsb), (Asb2, Bsb2, Rsb2)]
                    for _ in pre_gen(order[0], *bufs[0]):
                        pass
                    for i, ch in enumerate(order):
                        gens = [ser_gen(ch, *bufs[i % 2])]
                        if i + 1 < len(order):
                            gens.append(pre_gen(order[i + 1], *bufs[(i + 1) % 2]))
                        while gens:
                            for g_ in list(gens):
                                try:
                                    next(g_)
                                except StopIteration:
                                    gens.remove(g_)
                gT, bvT = big1[:, 0:L], big1[:, L:2 * L]
                E2r, t1r = big2[:, 0:L], big2[:, L:2 * L]
                k.dma("sp", gT, self.fg[cs, :], [self.fg.sub(c)], [big1])
                k.dma("act", bvT, self.fbv[cs, :], [self.fbv.sub(c)], [big1])
                for tb in range(nblk):
                    t0 = tb * 512
                    n = min(512, L - t0)
                    sl = slice(t0, t0 + n)
                    k.mm(ps[0][:, 0:n], self.bones[:], yacc[:, sl], True, True, [self.bones, yacc], [ps[0]])
                    k.stt(E1[:, sl], ps[0][:, 0:n], -1.0 / 64, yacc[:, sl], ALU.mult, ALU.add, [ps[0], yacc], [E1])
                    k.tt("pool", E2r[:, sl], E1[:, sl], E1[:, sl], ALU.mult, [E1], [big2])
                    k.mm(ps[1][:, 0:n], self.bones[:], E2r[:, sl], True, True, [self.bones, big2], [ps[1]])
                    k.act(t1r[:, sl], ps[1][:, 0:n], AF.Sqrt, [ps[1], self.eps_gn], [big2], bias=self.eps_gn[:], scale=1.0 / 64)
                k.op("dve", lambda e: e.reciprocal(t1r, t1r), [big2], [big2])
                k.tt("dve", E1[:], E1[:], t1r, ALU.mult, [E1, big2], [E1])
                k.ts("dve", E1[:], E1[:], vec[:, 84 + c:85 + c], vec[:, 90 + c:91 + c], ALU.mult, ALU.add, [E1, vec], [E1])
                k.tt("dve", E1[:], E1[:], bvT, ALU.add, [E1, big1], [E1])
                k.tt("dve", yo[:], E1[:], gT, ALU.mult, [E1, big1], [yo])
                k.dma("pool", self.yT[cs, :], yo[:], [yo], [self.yT.sub(("a", c))])

    def decl_moe(self):
        cfg, k = self.cfg, self.k
        DEP, L, NB = cfg.depth, cfg.L, cfg.nb
        self.CAP = cfg.cap
        self.NSLOT = 32 * self.CAP
        self.T = NB * L
        self.NTT = NB * cfg.NT
        self.w_branch = self.inp("w_branch", [DEP * D, D])
        self.w_out = self.inp("w_out", [DEP * D, D])
        self.ln_g = self.inp("ln_g", [DEP * 2, D])
        self.ln_b = self.inp("ln_b", [DEP * 2, D])
        self.w_r = self.inp("w_r", [DEP * 128, KD * 36])
        self.b_r = self.inp("b_r", [DEP, 36])
        self.w1r = self.inp("w1r", [DEP * 32 * 128, 8192])
        self.w3r = self.inp("w3r", [DEP * 32 * 128, 8192])
        self.w2r = self.inp("w2r", [DEP * 32 * 128, 8192])
        self.triu_in = self.inp("triu", [128, 128])
        self.ecap_in = self.inp("ecap", [128, 32])
        self.cnt_out = self.scratch("cnt", [DEP * 128, 32])
        self.zT = self.scratch("zT", [D, L], BF16)
        self.xslots = self.scratch("xslots", [self.NSLOT, D], BF16)
        self.yslots = [self.scratch(f"yslots{i}", [self.NSLOT // 2, D], F32) for i in range(2)]
        self.breg_h = self.nc.gpsimd.to_reg(self.NSLOT // 2 - 1)
        self.slotB_sb = k.sb("slotB_sb", [128, 2 * (cfg.nb * cfg.NT)], I32)
        self.triu = k.sb("triu_sb", [128, 128], F32)
        k.dma("sp", self.triu[:], self.triu_in.ap(), [self.triu_in], [self.triu])
        self.ecap = k.sb("ecap_sb", [128, 32], F32)
        k.dma("sp", self.ecap[:], self.ecap_in.ap(), [self.ecap_in], [self.ecap])
        self.breg = self.nc.gpsimd.to_reg(self.NSLOT - 1)
        self.off = k.sb("moe_off", [128, 32], F32)
        self.slot_sb = k.sb("slot_sb", [128, 2 * self.NTT], I32)
        self.gate_sb = k.sb("gate_sb", [128, 2 * self.NTT], F32)

    def load_w_bf(self, ph, wb, w_dram, row0, stage, nchunks=16):
        k = self.k
        for kc in range(nchunks):
            st = stage[kc % 2]
            k.dma("sp" if kc % 2 == 0 else "act", st[:], w_dram[row0 + kc * 128:row0 + (kc + 1) * 128, :], [w_dram], [st])
            k.copy(self.alt(("dve", "pool")), wb[:, kc, :], st[:], [st], [wb])

    def phase_merge(self, l, b):
        cfg, k, ps, pT = self.cfg, self.k, self.ps, self.pT
        L = cfg.L
        g0 = EXT_SEC["gate"][0]
        with self.phase(f"mrg{l}_{b}") as ph:
            wb = ph.sb("wb", [128, KD, D], BF16)
            with self.phase(f"ldwb{l}_{b}") as p2:
                stage = [p2.sb(f"stg{i}", [128, D], F32) for i in range(2)]
                self.load_w_bf(p2, wb, self.w_branch, l * D, stage)
            yblk = ph.sb("yblk", [128, KD, 512], BF16)
            zblk = ph.sb("zblk", [128, KD, 512], BF16)
            gt = [ph.sb(f"gt{i}", [128, 3, 512], F32) for i in range(2)]
            za = ph.sb("za", [128, 512], F32)
            zb = ph.sb("zb", [128, 512], F32)
            t0 = 0
            while t0 < L:
                n = min(512, L - t0)
                k.dma("sp", yblk[:, :, 0:n], self.yT[:, t0:t0 + n].rearrange("(k p) n -> p k n", p=128), [self.yT], [yblk])
                for nt in range(16):
                    g = gt[nt % 2]
                    for br in range(3):
                        r0 = g0 + br * D + nt * 128
                        k.dma("act" if br % 2 else "sp", g[:, br, 0:n], pT[r0:r0 + 128, t0:t0 + n], [pT.sub(r0 // 128)], [g])
                    k.act(g[:, :, 0:n], g[:, :, 0:n], AF.Sigmoid, [g], [g])
                    pa, pb_, pc = ps[(nt % 2) * 3 + 0], ps[(nt % 2) * 3 + 1], ps[(nt % 2) * 3 + 2]
                    for (pst, k0, k1) in ((pa, 0, 6), (pb_, 6, 12), (pc, 12, 16)):
                        for kc in range(k0, k1):
                            k.mm(pst[:, 0:n], wb[:, kc, nt * 128:(nt + 1) * 128], yblk[:, kc, 0:n], kc == k0, kc == k1 - 1,
                                 [wb, yblk], [pst])
                    k.tt("dve", za[:, 0:n], pa[:, 0:n], g[:, 0, 0:n], ALU.mult, [pa, g], [za])
                    k.tt("dve", zb[:, 0:n], pb_[:, 0:n], g[:, 1, 0:n], ALU.mult, [pb_, g], [zb])
                    k.tt("pool", za[:, 0:n], za[:, 0:n], zb[:, 0:n], ALU.add, [za, zb], [za])
                    k.tt("dve", zb[:, 0:n], pc[:, 0:n], g[:, 2, 0:n], ALU.mult, [pc, g], [zb])
                    k.tt("pool", zblk[:, nt, 0:n], za[:, 0:n], zb[:, 0:n], ALU.add, [za, zb], [zblk])
                k.dma("pool", self.zT[:, t0:t0 + n].rearrange("(k p) n -> p k n", p=128), zblk[:, :, 0:n], [zblk], [self.zT])
                t0 += n

    def ln_affine(self, st3, x_t, eps_tile, g_bc, b_bc):
        k = self.k
        self.ln_stats(st3, x_t, eps_tile)
        k.ts("dve", x_t[:], x_t[:], st3[1][:, 0:1], st3[2][:, 0:1], ALU.subtract, ALU.mult, [x_t, st3[1], st3[2]], [x_t])
        k.tt("pool", x_t[:], x_t[:], g_bc[:], ALU.mult, [x_t, g_bc], [x_t])
        k.tt("dve", x_t[:], x_t[:], b_bc[:], ALU.add, [x_t, b_bc], [x_t])

    def phase_out(self, l, b):
        cfg, k, ps = self.cfg, self.k, self.ps
        L, C, NT, NB = cfg.L, cfg.C, cfg.NT, cfg.nb
        CAP, NSLOT = self.CAP, self.NSLOT
        with self.phase(f"out{l}_{b}") as ph:
            wo = ph.sb("wo", [128, KD, D], BF16)
            with self.phase(f"ldwo{l}_{b}") as p2:
                stage = [p2.sb(f"stg{i}", [128, D], F32) for i in range(2)]
                self.load_w_bf(p2, wo, self.w_out, l * D, stage)
            bc = {}
            for nm, r, j, one in (("m2", b, 2, False), ("m2c", NB, 2, False), ("s3", b, 3, False), ("s3c", NB, 3, False),
                                  ("s4", b, 4, True), ("s4c", NB, 4, True)):
                bc[nm] = ph.sb(nm, [128, D], F32)
                self.load_bc(bc[nm], l, r, j, one)
            lng = ph.sb("lng", [128, D], F32)
            lnb = ph.sb("lnb", [128, D], F32)
            k.dma("sp", lng[:], self.ln_g[l * 2:l * 2 + 1, :].partition_broadcast(128), [self.ln_g], [lng])
            k.dma("act", lnb[:], self.ln_b[l * 2:l * 2 + 1, :].partition_broadcast(128), [self.ln_b], [lnb])
            wr = ph.sb("wr", [128, KD * 36], F32)
            k.dma("sp", wr[:], self.w_r[l * 128:(l + 1) * 128, :], [self.w_r], [wr])
            br = ph.sb("br", [128, 36], F32)
            k.dma("sp", br[:], self.b_r[l:l + 1, :].partition_broadcast(128), [self.b_r], [br])
            st3 = (ph.sb("stats", [128, 4, 6], F32), ph.sb("mv", [128, 2], F32), ph.sb("rstd", [128, 1], F32))
            zblk = ph.sb("zblk", [128, KD, 512], BF16)
            xt = ph.sb("xt", [128, D], F32)
            tm = ph.sb("tm", [128, D], F32)
            vt = ph.sb("vt", [128, D], F32)
            vb = ph.sb("vb", [128, D], BF16)
            vT = ph.sb("vT", [128, KD, 128], F32)
            sm = {nm: ph.sb(nm, shp, F32) for nm, shp in (
                ("lg", [128, 36]), ("gmax", [128, 1]), ("ngmax", [128, 1]), ("ohg", [128, 4]), ("e4", [128, 4]),
                ("pg", [128, 1]), ("prod", [128, 32]), ("lsel", [128, 8]), ("m8", [128, 8]), ("oh1", [128, 8]),
                ("oh2", [128, 8]), ("g1", [128, 1]), ("o32a", [128, 32]), ("o32b", [128, 32]), ("mask", [128, 32]),
                ("pos", [128, 32]), ("ovf", [128, 32]), ("slotf", [128, 2]), ("tmp32", [128, 32]))}
            if b == 0:
                k.op("dve", lambda e: e.memset(self.off[:], 0.0), [], [self.off])
            for t in range(NT):
                if t % 4 == 0:
                    n = min(512, L - t * 128)
                    k.dma("sp", zblk[:, :, 0:n], self.zT[:, t * 128:t * 128 + n].rearrange("(k p) n -> p k n", p=128), [self.zT], [zblk])
                isctx = t * 128 < C
                sfx = "c" if isctx else ""
                tl = slice((t % 4) * 128, (t % 4 + 1) * 128)
                k.dma("act", xt[:], self.xs[b][t * 128:(t + 1) * 128, :], [self.xs[b]], [xt])
                for nb_ in range(4):
                    pst = ps[nb_]
                    for kc in range(KD):
                        k.mm(pst[:, :], zblk[:, kc, tl], wo[:, kc, nb_ * 512:(nb_ + 1) * 512], kc == 0, kc == KD - 1, [zblk, wo], [pst])
                    k.tt("dve", tm[:, nb_ * 512:(nb_ + 1) * 512], pst[:, :], bc["m2" + sfx][:, nb_ * 512:(nb_ + 1) * 512], ALU.mult,
                         [pst, bc["m2" + sfx]], [tm])
                k.stt(xt[:], xt[:], DN_ALPHA, tm[:], ALU.mult, ALU.add, [xt, tm], [xt])
                self.ln_affine(st3, xt, self.eps_ln, lng, lnb)
                k.dma("pool", self.xs[b][t * 128:(t + 1) * 128, :], xt[:], [xt], [self.xs[b]])
                self.ln_stats(st3, xt, self.eps_ada)
                k.ts("dve", vt[:], xt[:], st3[1][:, 0:1], st3[2][:, 0:1], ALU.subtract, ALU.mult, [xt, st3[1], st3[2]], [vt])
                k.tt("pool", vt[:], vt[:], bc["s4" + sfx][:], ALU.mult, [vt, bc["s4" + sfx]], [vt])
                k.tt("dve", vt[:], vt[:], bc["s3" + sfx][:], ALU.add, [vt, bc["s3" + sfx]], [vt])
                k.copy("pool", vb[:], vt[:], [vt], [vb])
                for g4 in range(4):
                    pst = ps[4 + g4 % 2]
                    for j in range(4):
                        kc = g4 * 4 + j
                        k.tr(pst[:, j * 128:(j + 1) * 128], vt[:, kc * 128:(kc + 1) * 128], self.identf[:], [vt, self.identf], [pst])
                    k.copy(self.alt(), vT[:, g4 * 4:(g4 + 1) * 4, :], pst[:, :].rearrange("p (j n) -> p j n", j=4), [pst], [vT])
                for kc in range(KD):
                    k.mm(ps[6][:, 0:36], vT[:, kc, :], wr[:, kc * 36:(kc + 1) * 36], kc == 0, kc == KD - 1, [vT, wr], [ps[6]])
                s = sm
                k.tt("dve", s["lg"][:], ps[6][:, 0:36], br[:], ALU.add, [ps[6], br], [s["lg"]])
                k.op("dve", lambda e: e.tensor_reduce(s["gmax"][:], s["lg"][:, 0:4], AX.X, ALU.max), [s["lg"]], [s["gmax"]])
                k.ts("dve", s["ngmax"][:], s["gmax"][:], -1.0, None, ALU.mult, None, [s["gmax"]], [s["ngmax"]])
                k.ts("dve", s["ohg"][:], s["lg"][:, 0:4], s["gmax"][:, 0:1], None, ALU.is_equal, None, [s["lg"], s["gmax"]], [s["ohg"]])
                k.act(s["e4"][:], s["lg"][:, 0:4], AF.Exp, [s["lg"], s["ngmax"]], [s["e4"]], bias=s["ngmax"][:], scale=1.0)
                k.op("dve", lambda e: e.tensor_reduce(s["pg"][:], s["e4"][:], AX.X, ALU.add), [s["e4"]], [s["pg"]])
                k.op("dve", lambda e: e.reciprocal(s["pg"][:], s["pg"][:]), [s["pg"]], [s["pg"]])
                k.tt("dve", s["prod"][:].rearrange("p (g e) -> p g e", g=4), s["lg"][:, 4:36].rearrange("p (g e) -> p g e", g=4),
                     s["ohg"][:].rearrange("p (g o) -> p g o", o=1).broadcast_to([128, 4, 8]), ALU.mult, [s["lg"], s["ohg"]], [s["prod"]])
                k.op("dve", lambda e: e.tensor_reduce(s["lsel"][:], s["prod"][:].rearrange("p (g e) -> p e g", g=4), AX.X, ALU.add),
                     [s["prod"]], [s["lsel"]])
                k.op("dve", lambda e: e.max(s["m8"][:], s["lsel"][:]), [s["lsel"]], [s["m8"]])
                k.ts("dve", s["oh1"][:], s["lsel"][:], s["m8"][:, 0:1], None, ALU.is_equal, None, [s["lsel"], s["m8"]], [s["oh1"]])
                k.ts("dve", s["oh2"][:], s["lsel"][:], s["m8"][:, 1:2], None, ALU.is_equal, None, [s["lsel"], s["m8"]], [s["oh2"]])
                k.tt("dve", s["g1"][:], s["m8"][:, 0:1], s["m8"][:, 1:2], ALU.subtract, [s["m8"]], [s["g1"]])
                k.act(s["g1"][:], s["g1"][:], AF.Sigmoid, [s["g1"]], [s["g1"]])
                tt_i = b * NT + t
                gs = self.gate_sb
                k.tt("dve", gs[:, 2 * tt_i:2 * tt_i + 1], s["pg"][:], s["g1"][:], ALU.mult, [s["pg"], s["g1"]], [gs])
                k.tt("dve", gs[:, 2 * tt_i + 1:2 * tt_i + 2], s["pg"][:], gs[:, 2 * tt_i:2 * tt_i + 1], ALU.subtract, [s["pg"], gs], [gs])
                for nm, oh in (("o32a", "oh1"), ("o32b", "oh2")):
                    k.tt("dve", s[nm][:].rearrange("p (g e) -> p g e", g=4),
                         s["ohg"][:].rearrange("p (g o) -> p g o", o=1).broadcast_to([128, 4, 8]),
                         s[oh][:].rearrange("p (o e) -> p o e", o=1).broadcast_to([128, 4, 8]), ALU.mult, [s["ohg"], s[oh]], [s[nm]])
                k.tt("dve", s["mask"][:], s["o32a"][:], s["o32b"][:], ALU.add, [s["o32a"], s["o32b"]], [s["mask"]])
                k.mm(ps[7][:, 0:32], self.triu[:], s["mask"][:], True, True, [self.triu, s["mask"]], [ps[7]])
                k.mm(ps[7][:, 32:64], self.ones_f[:], s["mask"][:], True, True, [self.ones_f, s["mask"]], [ps[7]])
                k.tt("dve", s["pos"][:], ps[7][:, 0:32], s["mask"][:], ALU.subtract, [ps[7], s["mask"]], [s["pos"]])
                k.tt("dve", s["pos"][:], s["pos"][:], self.off[:], ALU.add, [s["pos"], self.off], [s["pos"]])
                k.ts("dve", s["ovf"][:], s["pos"][:], float(CAP), float(1 << 24), ALU.is_ge, ALU.mult, [s["pos"]], [s["ovf"]])
                k.tt("dve", s["pos"][:], s["pos"][:], s["ovf"][:], ALU.add, [s["pos"], s["ovf"]], [s["pos"]])
                k.tt("dve", s["pos"][:], s["pos"][:], self.ecap[:], ALU.add, [s["pos"], self.ecap], [s["pos"]])
                for kk_, nm in ((0, "o32a"), (1, "o32b")):
                    k.tt("dve", s["tmp32"][:], s["pos"][:], s[nm][:], ALU.mult, [s["pos"], s[nm]], [s["tmp32"]])
                    k.op("dve", lambda e, kk_=kk_: e.tensor_reduce(s["slotf"][:, kk_:kk_ + 1], s["tmp32"][:], AX.X, ALU.add),
                         [s["tmp32"]], [s["slotf"]])
                k.copy("dve", self.slot_sb[:, 2 * tt_i:2 * tt_i + 2], s["slotf"][:], [s["slotf"]], [self.slot_sb])
                k.ts("dve", s["slotf"][:], s["slotf"][:], -float(NSLOT // 2), None, ALU.add, None, [s["slotf"]], [s["slotf"]])
                k.copy("dve", self.slotB_sb[:, 2 * tt_i:2 * tt_i + 2], s["slotf"][:], [s["slotf"]], [self.slotB_sb])
                k.tt("dve", self.off[:], self.off[:], ps[7][:, 32:64], ALU.add, [self.off, ps[7]], [self.off])
                for kk_ in range(2):
                    k.idma(self.xslots.ap(), bass.IndirectOffsetOnAxis(ap=self.slot_sb[:, 2 * tt_i + kk_:2 * tt_i + kk_ + 1], axis=0),
                           vb[:], None, [vb, self.slot_sb], [self.xslots], bounds_check=self.breg, oob_is_err=False)

    def phase_experts(self, l):
        cfg, k, ps = self.cfg, self.k, self.ps
        CAP = self.CAP
        groups = []
        s0 = 0
        while s0 < CAP:
            groups.append((s0, min(512, CAP - s0)))
            s0 += 512
        with self.phase(f"exp{l}") as ph:
            stage = [ph.sb(f"stg{i}", [128, 8192], F32) for i in range(2)]
            w1b = ph.sb("w1b", [128, KD, 512], BF16)
            w3b = ph.sb("w3b", [128, KD, 512], BF16)
            w2b = ph.sb("w2b", [128, 4, D], BF16)
            xb = [ph.sb(f"xb{i}", [128, D], BF16) for i in range(2)]
            xbT = ph.sb("xbT", [128, KD, 512], BF16)
            hT = ph.sb("hT", [128, 4, 512], BF16)
            h1 = [ph.sb(f"h1{i}", [128, 512], F32) for i in range(2)]
            yt = [ph.sb(f"yt{i}", [128, D], F32) for i in range(2)]
            si = 0
            for e in range(32):
                r0 = (l * 32 + e) * 128
                for (wsrc, wdst) in ((self.w1r, w1b), (self.w3r, w3b), (self.w2r, w2b)):
                    st = stage[si % 2]
                    k.dma("sp" if si % 2 == 0 else "act", st[:], wsrc[r0:r0 + 128, :], [wsrc], [st])
                    wflat = wdst[:].rearrange("p a n -> p (a n)")
                    k.copy("dve", wflat[:, 0:4096], st[:, 0:4096], [st], [wdst])
                    k.copy("pool", wflat[:, 4096:8192], st[:, 4096:8192], [st], [wdst])
                    si += 1
                for (g0, gn) in groups:
                    nst = gn // 128
                    for st_ in range(nst):
                        x_b = xb[st_ % 2]
                        row = e * CAP + g0 + st_ * 128
                        k.dma("sp", x_b[:], self.xslots[row:row + 128, :], [self.xslots], [x_b])
                        for g4 in range(4):
                            pst = ps[4 + (st_ * 4 + g4) % 4]
                            pv = pst.ap().bitcast(BF16)
                            for j in range(4):
                                kc = g4 * 4 + j
                                k.tr(pv[:, j * 128:(j + 1) * 128], x_b[:, kc * 128:(kc + 1) * 128], self.identb[:], [x_b, self.identb], [pst])
                            k.copy(self.alt(), xbT[:, g4 * 4:(g4 + 1) * 4, st_ * 128:(st_ + 1) * 128],
                                   pv[:, 0:512].rearrange("p (j n) -> p j n", j=4), [pst], [xbT])
                    for f in range(4):
                        p1, p3 = ps[(f % 2) * 2], ps[(f % 2) * 2 + 1]
                        for kc in range(KD):
                            k.mm(p1[:, 0:gn], w1b[:, kc, f * 128:(f + 1) * 128], xbT[:, kc, 0:gn], kc == 0, kc == KD - 1, [w1b, xbT], [p1])
                        for kc in range(KD):
                            k.mm(p3[:, 0:gn], w3b[:, kc, f * 128:(f + 1) * 128], xbT[:, kc, 0:gn], kc == 0, kc == KD - 1, [w3b, xbT], [p3])
                        hh = h1[f % 2]
                        k.act(hh[:, 0:gn], p1[:, 0:gn], AF.Silu, [p1], [hh])
                        k.tt("dve", hT[:, f, 0:gn], hh[:, 0:gn], p3[:, 0:gn], ALU.mult, [hh, p3], [hT])
                    for st_ in range(nst):
                        y_t = yt[st_ % 2]
                        for nb_ in range(4):
                            pst = ps[4 + nb_]
                            for f in range(4):
                                k.mm(pst[:, :], hT[:, f, st_ * 128:(st_ + 1) * 128], w2b[:, f, nb_ * 512:(nb_ + 1) * 512], f == 0, f == 3,
                                     [hT, w2b], [pst])
                            k.copy(self.alt(), y_t[:, nb_ * 512:(nb_ + 1) * 512], pst[:, :], [pst], [y_t])
                        row = e * CAP + g0 + st_ * 128
                        ys = self.yslots[row // (self.NSLOT // 2)]
                        row = row % (self.NSLOT // 2)
                        k.dma("pool", ys[row:row + 128, :], y_t[:], [y_t], [ys])

    def phase_combine(self, l, b):
        cfg, k, ps = self.cfg, self.k, self.ps
        L, C, S, NT, NB = cfg.L, cfg.C, cfg.S, cfg.NT, cfg.nb
        last = l == cfg.depth - 1
        with self.phase(f"cmb{l}_{b}") as ph:
            bc = {}
            for nm, r in (("m5", b), ("m5c", NB)):
                bc[nm] = ph.sb(nm, [128, D], F32)
                self.load_bc(bc[nm], l, r, 5, False)
            lng = ph.sb("lng", [128, D], F32)
            lnb = ph.sb("lnb", [128, D], F32)
            k.dma("sp", lng[:], self.ln_g[l * 2 + 1:l * 2 + 2, :].partition_broadcast(128), [self.ln_g], [lng])
            k.dma("act", lnb[:], self.ln_b[l * 2 + 1:l * 2 + 2, :].partition_broadcast(128), [self.ln_b], [lnb])
            st3 = (ph.sb("stats", [128, 4, 6], F32), ph.sb("mv", [128, 2], F32), ph.sb("rstd", [128, 1], F32))
            y0 = [ph.sb(f"y0{i}", [128, D], F32) for i in range(2)]
            y1 = [ph.sb(f"y1{i}", [128, D], F32) for i in range(2)]
            xt = [ph.sb(f"xt{i}", [128, D], F32) for i in range(2)]
            for t in range(NT):
                isctx = t * 128 < C
                if last and isctx:
                    continue
                tt_i = b * NT + t
                ya, yb_, x_t = y0[t % 2], y1[t % 2], xt[t % 2]
                for kk_, yy in ((0, ya), (1, yb_)):
                    k.op("pool", lambda e, yy=yy: e.memset(yy[:], 0.0), [], [yy])
                    k.idma(yy[:], None, self.yslots[0].ap(),
                           bass.IndirectOffsetOnAxis(ap=self.slot_sb[:, 2 * tt_i + kk_:2 * tt_i + kk_ + 1], axis=0),
                           [self.yslots[0], self.slot_sb], [yy], bounds_check=self.breg_h, oob_is_err=False)
                    k.idma(yy[:], None, self.yslots[1].ap(),
                           bass.IndirectOffsetOnAxis(ap=self.slotB_sb[:, 2 * tt_i + kk_:2 * tt_i + kk_ + 1], axis=0),
                           [self.yslots[1], self.slotB_sb], [yy], bounds_check=self.breg_h, oob_is_err=False)
                k.dma("sp", x_t[:], self.xs[b][t * 128:(t + 1) * 128, :], [self.xs[b]], [x_t])
                k.ts("dve", ya[:], ya[:], self.gate_sb[:, 2 * tt_i:2 * tt_i + 1], None, ALU.mult, None, [ya, self.gate_sb], [ya])
                k.stt(ya[:], yb_[:], self.gate_sb[:, 2 * tt_i + 1:2 * tt_i + 2], ya[:], ALU.mult, ALU.add, [yb_, self.gate_sb, ya], [ya])
                k.tt("pool", ya[:], ya[:], bc["m5c" if isctx else "m5"][:], ALU.mult, [ya, bc["m5c" if isctx else "m5"]], [ya])
                k.stt(x_t[:], x_t[:], DN_ALPHA, ya[:], ALU.mult, ALU.add, [x_t, ya], [x_t])
                self.ln_affine(st3, x_t, self.eps_ln, lng, lnb)
                if last:
                    r0 = b * S + t * 128 - C
                    k.dma("pool", self.out[r0:r0 + 128, :], x_t[:], [x_t], [self.out])
                else:
                    k.dma("pool", self.xs[b][t * 128:(t + 1) * 128, :], x_t[:], [x_t], [self.xs[b]])

    def finish(self):
        self.k.barrier()
        return self


def prep_shared(cfg, inputs):
    DEP = cfg.depth
    sh = {}
    w_mod = np.asarray(inputs["w_mod"])[:DEP]
    sh["w_mod"] = np.ascontiguousarray(
        w_mod.reshape(DEP, KD, 128, 24, 512).transpose(0, 3, 2, 1, 4)).reshape(DEP * 24 * 128, KD * 512)
    sh["b_mod"] = np.ascontiguousarray(np.asarray(inputs["b_mod"])[:DEP])
    w_in = np.asarray(inputs["w_in"])[:DEP]
    w_e = w_in[:, :, EXT_COLS]
    sh["w_ext"] = np.ascontiguousarray(
        w_e.reshape(DEP, KD, 128, NT_EXT, 128).transpose(0, 3, 2, 1, 4)).reshape(DEP * NT_EXT * 128, KD * 128)
    sh["ident_f"] = np.eye(128, dtype=np.float32)
    S, C, L = cfg.S, cfg.C, cfg.L
    pos = np.arange(S)
    rowp = (pos // 64).astype(np.float32)
    colp = (pos % 64).astype(np.float32)
    inv = (np.float32(10000.0) ** (-np.arange(16, dtype=np.float32) / np.float32(16))).astype(np.float32)
    d = np.arange(64)
    axis, half, f = d // 32, (d % 32) // 16, d % 16
    ang = np.where(axis[:, None] == 0, rowp[None, :], colp[None, :]).astype(np.float32) * inv[f][:, None]
    cosT = np.ones((64, L), np.float32)
    sinT = np.zeros((64, L), np.float32)
    cosT[:, C:] = np.cos(ang)
    sinT[:, C:] = np.sin(ang) * np.where(half == 0, -1.0, 1.0)[:, None]
    sh["ropec"] = np.ascontiguousarray(np.concatenate([cosT, cosT], 0).astype(np.float32))
    sh["ropes"] = np.ascontiguousarray(np.concatenate([sinT, sinT], 0).astype(np.float32))
    kk_, qq_ = np.arange(128)[:, None], np.arange(128)[None, :]
    sh["maskP"] = (qq_ <= kk_).astype(np.float32)
    sh["maskN"] = (kk_ <= qq_).astype(np.float32)
    def chT(v, nt):
        return np.asarray(v).reshape(nt, 128).T
    rv = np.zeros((DEP, 128, 96), np.float32)
    for l in range(DEP):
        mu = np.asarray(inputs["rwkv_mu"])[l]
        rv[l, :, 0:21] = chT(mu[0], 21)
        rv[l, :, 21:42] = chT(mu[1], 21)
        w0 = np.asarray(inputs["rwkv_w0"])[l]
        a0 = np.asarray(inputs["rwkv_a0"])[l]
        kv = np.asarray(inputs["rwkv_kvec"])[l]
        lnx = np.asarray(inputs["rwkv_lnx"])[l]
        for d_ in range(2):
            rv[l, :, 42 + d_ * 6:48 + d_ * 6] = chT(w0[d_], 6)
            rv[l, :, 54 + d_ * 6:60 + d_ * 6] = chT(a0[d_], 6)
        rv[l, :, 66:72] = chT(kv[0], 6)
        rv[l, :, 72:78] = chT(kv[1], 6)
        rv[l, :, 78:84] = chT(kv[2], 6)
        rv[l, :, 84:90] = chT(lnx[0], 6)
        rv[l, :, 90:96] = chT(lnx[1], 6)
    sh["rwvec"] = rv.reshape(DEP * 128, 96)
    sh["w_up"] = np.ascontiguousarray(np.asarray(inputs["rwkv_w_up"])[:DEP].reshape(DEP * 128, 768))
    sh["a_up"] = np.ascontiguousarray(np.asarray(inputs["rwkv_a_up"])[:DEP].reshape(DEP * 128, 768))
    sh["g_up"] = np.ascontiguousarray(np.asarray(inputs["rwkv_g_up"])[:DEP].reshape(DEP * 128, 768))
    s_, t_ = np.arange(64)[:, None], np.arange(64)[None, :]
    MS, MI = (s_ < t_).astype(np.float32), (s_ <= t_).astype(np.float32)
    mA_f = np.concatenate([MS, MS, MS.T, MS.T, MS, MS, MI, MI], axis=1)
    mA_b = np.concatenate([MS.T, MS.T, MS, MS, MS.T, MS.T, MI.T, MI.T], axis=1)
    sh["maskA"] = np.ascontiguousarray(np.concatenate([mA_f, mA_f, mA_b, mA_b], axis=0))
    mB_f = -np.concatenate([MI, MI], axis=1)
    mB_b = -np.concatenate([MI.T, MI.T], axis=1)
    sh["maskB"] = np.ascontiguousarray(np.concatenate([mB_f, mB_f, mB_b, mB_b], axis=0))
    e64 = np.eye(64, dtype=np.float32)
    sh["ident2"] = np.ascontiguousarray(np.tile(np.concatenate([e64, e64], axis=1), (2, 1)))
    bo = np.zeros((128, 128), np.float32); bo[:64, :64] = 1; bo[64:, 64:] = 1
    sh["bones"] = bo
    rm = np.ones((128, L), np.float32); rm[:, ::64] = 0
    sh["resetm"] = rm
    sh["w_branch"] = np.ascontiguousarray(np.asarray(inputs["w_branch"])[:DEP].reshape(DEP * D, D))
    sh["w_out"] = np.ascontiguousarray(np.asarray(inputs["w_out"])[:DEP].reshape(DEP * D, D))
    sh["ln_g"] = np.ascontiguousarray(np.asarray(inputs["ln_g"])[:DEP].reshape(DEP * 2, D))
    sh["ln_b"] = np.ascontiguousarray(np.asarray(inputs["ln_b"])[:DEP].reshape(DEP * 2, D))
    wr_ = np.concatenate([np.asarray(inputs["w_rg"])[:DEP], np.asarray(inputs["w_re"])[:DEP]], axis=2)
    sh["w_r"] = np.ascontiguousarray(wr_.reshape(DEP, KD, 128, 36).transpose(0, 2, 1, 3)).reshape(DEP * 128, KD * 36)
    sh["b_r"] = np.ascontiguousarray(np.concatenate([np.asarray(inputs["b_rg"])[:DEP], np.asarray(inputs["b_re"])[:DEP]], axis=1))
    for nm, src_ in (("w1r", "w1"), ("w3r", "w3")):
        w = np.asarray(inputs[src_])[:DEP]
        sh[nm] = np.ascontiguousarray(w.reshape(DEP, 32, KD, 128, 512).transpose(0, 1, 3, 2, 4)).reshape(DEP * 32 * 128, 8192)
    w = np.asarray(inputs["w2"])[:DEP]
    sh["w2r"] = np.ascontiguousarray(w.reshape(DEP, 32, 4, 128, D).transpose(0, 1, 3, 2, 4)).reshape(DEP * 32 * 128, 8192)
    sh["triu"] = (np.arange(128)[:, None] <= np.arange(128)[None, :]).astype(np.float32)
    sh["ecap"] = np.tile((np.arange(32, dtype=np.float32) * cfg.cap)[None, :], (128, 1)).astype(np.float32)
    sh["diff_lam"] = np.ascontiguousarray(np.asarray(inputs["diff_lam"])[:DEP].reshape(DEP, 256))
    sh["diff_subln"] = np.ascontiguousarray(np.asarray(inputs["diff_subln"])[:DEP])
    sh["win_sink"] = np.ascontiguousarray(np.asarray(inputs["win_sink"])[:DEP])
    return sh


def prep_core(cfg, inputs, core):
    NB, S, C = cfg.nb, cfg.S, cfg.C
    b0 = core * NB
    m = {}
    m["xin"] = np.ascontiguousarray(np.asarray(inputs["x"])[b0:b0 + NB].reshape(NB * S, D))
    m["ctxin"] = np.ascontiguousarray(np.asarray(inputs["ctx"])[b0:b0 + NB].reshape(NB * C, D))
    cc = np.concatenate([np.asarray(inputs["c"])[b0:b0 + NB], np.asarray(inputs["c_ctx"])[None, :]], axis=0)
    NR = NB + 1
    m["ccT"] = np.ascontiguousarray(cc.reshape(NR, KD, 128).transpose(2, 1, 0)).reshape(128, KD * NR)
    return m


def run(cfg, inputs, trace=False):
    prog = Prog(cfg).build()
    shared = prep_shared(cfg, inputs)
    in_maps = []
    for c in range(cfg.ncores):
        m = dict(shared)
        m.update(prep_core(cfg, inputs, c))
        in_maps.append({n: m[n] for n in prog.inputs})
    res = run_bass_kernel_spmd(prog.nc, in_maps, core_ids=list(range(cfg.ncores)), trace=trace)
    return prog, res


N_CORES = 4


def kernel(**inputs):
    nb = 16 // N_CORES
    cap = int(math.ceil(3.1 * 2 * nb * (2048 + 256) / 32 / 128.0)) * 128
    cfg = Cfg(ncores=N_CORES, nb=nb, S=2048, C=256, depth=4, cap=cap)
    prog, res = run(cfg, inputs)
    outs = [res.results[c]["out"].reshape(cfg.nb, cfg.S, D) for c in range(cfg.ncores)]
    return np.concatenate(outs, axis=0).astype(np.float32)
```

```python
import math
import numpy as np
import ml_dtypes
import concourse.bass as bass
import concourse.mybir as mybir
from concourse.bass_utils import run_bass_kernel_spmd

F32 = mybir.dt.float32
BF16 = mybir.dt.bfloat16
I32 = mybir.dt.int32
U32 = mybir.dt.uint32
AF = mybir.ActivationFunctionType
ALU = mybir.AluOpType
AX = mybir.AxisListType

D = 2048
KD = 16
RW = 768
DIFF_W = 768
WIN_W = 512
WIN_KV_W = 128
RWKV_IN = 2688
DIFF_OFF = 2688
WIN_OFF = DIFF_OFF + 2304
GATE_OFF = WIN_OFF + 768
N_IN = GATE_OFF + 3 * D
DEPTH_FULL = 4
DN_ALPHA = (2 * DEPTH_FULL) ** 0.25
ADA_EPS = 1e-6
LN_EPS = 1e-5


def _rope_partner(cols):
    cols = np.asarray(cols).reshape(-1, 64)
    d = np.arange(64)
    return cols[:, d ^ 16].reshape(-1)


def ext_cols():
    sec = {}
    out = []

    def add(name, c):
        c = np.asarray(c, dtype=np.int64)
        assert len(c) % 128 == 0, name
        sec[name] = (len(out_flat()), len(c))
        out.append(c)

    def out_flat():
        return np.concatenate(out) if out else np.zeros((0,), np.int64)

    add("rw", np.arange(0, RWKV_IN))
    dq = DIFF_OFF + np.arange(0, DIFF_W)
    dk = DIFF_OFF + DIFF_W + np.arange(0, DIFF_W)
    dv = DIFF_OFF + 2 * DIFF_W + np.arange(0, DIFF_W)
    add("dq", dq)
    add("dqp", _rope_partner(dq))
    add("dk", dk)
    add("dkp", _rope_partner(dk))
    add("dv", dv)
    wq = []
    for g in range(4):
        for hkv in range(2):
            wq.append(WIN_OFF + hkv * 256 + g * 64 + np.arange(64))
    wq = np.concatenate(wq)
    add("wq", wq)
    add("wqp", _rope_partner(wq))
    wk = WIN_OFF + WIN_W + np.arange(0, WIN_KV_W)
    add("wk", wk)
    add("wkp", _rope_partner(wk))
    add("wv", WIN_OFF + WIN_W + WIN_KV_W + np.arange(0, WIN_KV_W))
    add("gate", GATE_OFF + np.arange(0, 3 * D))
    return out_flat(), sec


EXT_COLS, EXT_SEC = ext_cols()
N_EXT = len(EXT_COLS)
NT_EXT = N_EXT // 128


class Cfg:
    def __init__(self, ncores=8, nb=2, S=2048, C=256, depth=4, dump=(), stop_after=None, dbg=99, cap=768):
        self.dbg = dbg
        self.cap = cap
        self.ncores = ncores
        self.nb = nb
        self.S = S
        self.C = C
        self.L = S + C
        self.depth = depth
        self.dump = tuple(dump)
        self.stop_after = stop_after
        self.NT = self.L // 128


class Buf:
    __slots__ = ("t", "last_w", "readers", "name", "subs")

    def __init__(self, t, name=""):
        self.t = t
        self.last_w = None
        self.readers = {}
        self.name = name
        self.subs = {}

    def sub(self, key):
        b = self.subs.get(key)
        if b is None:
            b = Buf(self.t, f"{self.name}.{key}")
            self.subs[key] = b
        return b

    def __getitem__(self, idx):
        return self.t[idx]

    def ap(self):
        return self.t.ap()


class KB:
    def __init__(self, nc, ndma=32):
        self.nc = nc
        self.E = {"pe": nc.tensor, "act": nc.scalar, "dve": nc.vector, "pool": nc.gpsimd, "sp": nc.sync}
        self.sem = {e: nc.alloc_semaphore(f"s_{e}") for e in self.E}
        self.cnt = {e: 0 for e in self.E}
        self.seen = {e: {} for e in self.E}
        self.ndma = ndma
        self.dsem = [nc.alloc_semaphore(f"d{i}") for i in range(ndma)]
        self.dcnt = [0] * ndma
        self.dnext = 0
        self.ninst = 0

    def sb(self, name, shape, dtype=F32):
        return Buf(self.nc.alloc_sbuf_tensor(name, list(shape), dtype), name)

    def psum(self, name, shape, dtype=F32):
        return Buf(self.nc.alloc_psum_tensor(name, list(shape), dtype), name)

    def dram(self, name, shape, dtype=F32, kind="Internal"):
        return Buf(self.nc.dram_tensor(name, list(shape), dtype, kind=kind), name)

    def _semobj(self, key):
        return self.sem[key] if isinstance(key, str) else self.dsem[key]

    def _wait(self, eng, key, val):
        if eng == "pe" and key == "pe":
            return
        if self.seen[eng].get(key, 0) >= val:
            return
        self.E[eng].wait_ge(self._semobj(key), val)
        self.seen[eng][key] = val
        self.ninst += 1

    def _deps(self, eng, reads, writes):
        for r in reads:
            if r.last_w is not None:
                self._wait(eng, *r.last_w)
        for w in writes:
            if w.last_w is not None:
                self._wait(eng, *w.last_w)
            for k, v in w.readers.items():
                self._wait(eng, k, v)

    def op(self, eng, fn, reads, writes):
        self._deps(eng, reads, writes)
        inst = fn(self.E[eng])
        self.cnt[eng] += 1
        c = self.cnt[eng]
        inst.then_inc(self.sem[eng], 1)
        self.ninst += 1
        for r in reads:
            if r.readers.get(eng, 0) < c:
                r.readers[eng] = c
        for w in writes:
            w.last_w = (eng, c)
            w.readers = {}
        return inst

    def _dma_common(self, q, reads, writes, emit):
        self._deps(q, reads, writes)
        s = self.dnext
        self.dnext = (s + 1) % self.ndma
        if self.dcnt[s] > 0:
            self._wait(q, s, 16 * self.dcnt[s])
        self.dcnt[s] += 1
        v = 16 * self.dcnt[s]
        emit().then_inc(self.dsem[s], 16)
        self.ninst += 1
        for r in reads:
            r.readers[s] = v
        for w in writes:
            w.last_w = (s, v)
            w.readers = {}

    def dma(self, q, out_ap, in_ap, reads, writes, **kw):
        self._dma_common(q, reads, writes, lambda: self.E[q].dma_start(out=out_ap, in_=in_ap, **kw))

    def idma(self, out_ap, out_off, in_ap, in_off, reads, writes, **kw):
        self._dma_common("pool", reads, writes,
                         lambda: self.nc.gpsimd.indirect_dma_start(out=out_ap, out_offset=out_off, in_=in_ap,
                                                                   in_offset=in_off, **kw))

    def barrier(self):
        for e in self.E:
            for e2 in self.E:
                if e2 != e and self.cnt[e2] > 0:
                    self._wait(e, e2, self.cnt[e2])
            for s in range(self.ndma):
                if self.dcnt[s] > 0:
                    self._wait(e, s, 16 * self.dcnt[s])

    def mm(self, out_ap, lhsT, rhs, start, stop, reads, writes, **kw):
        return self.op("pe", lambda e: e.matmul(out_ap, lhsT, rhs, start=start, stop=stop, **kw), reads, writes)

    def tr(self, out_ap, in_ap, ident_ap, reads, writes):
        return self.op("pe", lambda e: e.transpose(out_ap, in_ap, ident_ap), reads, writes)

    def act(self, out_ap, in_ap, func, reads, writes, **kw):
        return self.op("act", lambda e: e.activation(out_ap, in_ap, func, **kw), reads, writes)

    def copy(self, eng, out_ap, in_ap, reads, writes):
        if eng == "act":
            return self.op("act", lambda e: e.copy(out_ap, in_ap), reads, writes)
        return self.op(eng, lambda e: e.tensor_copy(out_ap, in_ap), reads, writes)

    def tt(self, eng, out_ap, a, b, op, reads, writes):
        return self.op(eng, lambda e: e.tensor_tensor(out_ap, a, b, op), reads, writes)

    def ts(self, eng, out_ap, a, s1, s2, op0, op1, reads, writes):
        if op1 is None:
            return self.op(eng, lambda e: e.tensor_scalar(out_ap, a, s1, None, op0), reads, writes)
        return self.op(eng, lambda e: e.tensor_scalar(out_ap, a, s1, s2, op0, op1), reads, writes)

    def stt(self, out_ap, a, s, b, op0, op1, reads, writes):
        return self.op("dve", lambda e: e.scalar_tensor_tensor(out_ap, a, s, b, op0, op1), reads, writes)


class Phase:
    _uid = [0]

    def __init__(self, prog, name):
        self.prog, self.name = prog, name

    def __enter__(self):
        from contextlib import ExitStack
        self.es = ExitStack()
        self.es.__enter__()
        return self

    def sb(self, name, shape, dtype=F32):
        Phase._uid[0] += 1
        nm = f"{self.name}_{name}_{Phase._uid[0]}"
        t = self.es.enter_context(self.prog.nc.sbuf_tensor(nm, list(shape), dtype))
        return Buf(t, nm)

    def __exit__(self, *a):
        if a[0] is None:
            self.prog.k.barrier()
        return self.es.__exit__(*a)


class Prog:
    def __init__(self, cfg):
        self.cfg = cfg
        self.nc = bass.Bass("TRN2", target_bir_lowering=False)
        self.k = KB(self.nc)
        self.inputs = {}
        self.outputs = {}
        self.rr = 0

    def inp(self, name, shape, dtype=F32):
        b = Buf(self.nc.dram_tensor(name, list(shape), dtype, kind="ExternalInput"), name)
        self.inputs[name] = (tuple(shape), dtype)
        return b

    def scratch(self, name, shape, dtype=F32):
        kind = "ExternalOutput" if name in self.cfg.dump else "Internal"
        b = Buf(self.nc.dram_tensor(name, list(shape), dtype, kind=kind), name)
        if kind == "ExternalOutput":
            self.outputs[name] = (tuple(shape), dtype)
        return b

    def alt(self, engines=("act", "dve")):
        self.rr += 1
        return engines[self.rr % len(engines)]

    def build(self):
        cfg, k, nc = self.cfg, self.k, self.nc
        NB, L, C, S, NT = cfg.nb, cfg.L, cfg.C, cfg.S, cfg.NT
        NR = NB + 1
        DEP = cfg.depth

        xin = self.inp("xin", [NB * S, D])
        ctxin = self.inp("ctxin", [NB * C, D])
        ccT = self.inp("ccT", [128, KD * NR])
        w_mod = self.inp("w_mod", [DEP * 24 * 128, KD * 512])
        b_mod = self.inp("b_mod", [DEP, 6 * D])
        w_ext = self.inp("w_ext", [DEP * NT_EXT * 128, KD * 128])
        ident_f = self.inp("ident_f", [128, 128])
        self.out = Buf(nc.dram_tensor("out", [NB * S, D], F32, kind="ExternalOutput"), "out")
        self.outputs["out"] = ((NB * S, D), F32)

        xs = [self.scratch(f"xs{b}", [L, D]) for b in range(NB)]
        modv = self.scratch("modv", [DEP * NR, 6 * D])
        pT = self.scratch("pT", [N_EXT, L])

        identf = k.sb("identf", [128, 128], F32)
        identb = k.sb("identb", [128, 128], BF16)
        k.dma("sp", identf[:], ident_f.ap(), [ident_f], [identf])
        k.copy("dve", identb[:], identf[:], [identf], [identb])
        ps = [k.psum(f"ps{i}", [128, 512], F32) for i in range(8)]
        self.ps = ps

        for b in range(NB):
            k.dma("sp", xs[b][0:C, :], ctxin[b * C:(b + 1) * C, :], [ctxin], [xs[b]])
            for s0 in range(0, S, 512):
                s1 = min(S, s0 + 512)
                k.dma("pool" if (s0 // 512) % 2 else "sp", xs[b][C + s0:C + s1, :], xin[b * S + s0:b * S + s1, :], [xin], [xs[b]])

        phm = Phase(self, "mod").__enter__()
        sc = phm.sb("mod_sc", [128, KD * NR], F32)
        k.dma("sp", sc[:], ccT.ap(), [ccT], [sc])
        k.act(sc[:], sc[:], AF.Silu, [sc], [sc])
        wm = [phm.sb(f"mod_w{i}", [128, KD * 512], F32) for i in range(2)]
        mo = [phm.sb(f"mod_o{i}", [NR, 512], F32) for i in range(2)]
        mb = [phm.sb(f"mod_b{i}", [NR, 512], F32) for i in range(2)]
        it = 0
        for l in range(DEP):
            for nb_ in range(24):
                w = wm[it % 2]
                o = mo[it % 2]
                bb = mb[it % 2]
                pst = ps[it % 2]
                r0 = (l * 24 + nb_) * 128
                k.dma("sp" if it % 2 == 0 else "sp", w[:], w_mod[r0:r0 + 128, :], [w_mod], [w])
                k.dma("pool", bb[:], b_mod[l:l + 1, nb_ * 512:(nb_ + 1) * 512].partition_broadcast(NR),
                      [b_mod], [bb])
                for kk in range(KD):
                    k.mm(pst[0:NR, :], sc[:, kk * NR:(kk + 1) * NR], w[:, kk * 512:(kk + 1) * 512],
                         kk == 0, kk == KD - 1, [sc, w], [pst])
                k.tt("dve", o[:], pst[0:NR, :], bb[:], ALU.add, [pst, bb], [o])
                k.dma("pool", modv[l * NR:(l + 1) * NR, nb_ * 512:(nb_ + 1) * 512], o[:], [o], [modv])
                it += 1
        phm.__exit__(None, None, None)
        if cfg.stop_after == "mod":
            return self.finish()

        self.xin, self.ctxin, self.xs, self.modv, self.pT = xin, ctxin, xs, modv, pT
        self.identf, self.identb = identf, identb
        self.w_ext = w_ext
        self.NR = NR
        self.decl_rest()
        self.decl_rwkv()
        self.decl_moe()
        for l in range(DEP):
            for b in range(NB):
                self.phase_proj(l, b)
                if cfg.stop_after == "proj":
                    return self.finish()
                if "noattn" not in cfg.dump:
                    self.phase_diff(l, b)
                    self.phase_win(l, b)
                if cfg.stop_after == "attn":
                    return self.finish()
                self.phase_rwkv_feat(l, b)
                if cfg.stop_after == "rwf":
                    return self.finish()
                self.phase_rwkv_scan(l, b)
                if cfg.stop_after == "rws":
                    return self.finish()
                self.phase_merge(l, b)
                if cfg.stop_after == "mrg":
                    return self.finish()
                self.phase_out(l, b)
                if cfg.stop_after == "out":
                    return self.finish()
            if "cnt" in cfg.dump:
                k.dma("sp", self.cnt_out[l * 128:(l + 1) * 128, :], self.off[:], [self.off], [self.cnt_out])
            self.phase_experts(l)
            for b in range(NB):
                self.phase_combine(l, b)
        return self.finish()

    def phase(self, name):
        return Phase(self, name)

    def decl_rest(self):
        cfg, k, nc = self.cfg, self.k, self.nc
        DEP, L = cfg.depth, cfg.L
        self.ropec = self.inp("ropec", [128, L])
        self.ropes = self.inp("ropes", [128, L])
        self.diff_lam = self.inp("diff_lam", [DEP, 256])
        self.diff_subln = self.inp("diff_subln", [DEP, 128])
        self.win_sink = self.inp("win_sink", [DEP, 8])
        self.maskP_in = self.inp("maskP", [128, 128])
        self.maskN_in = self.inp("maskN", [128, 128])
        self.yT = self.scratch("yT", [D, L], BF16)
        self.ones_bf = k.sb("ones_bf", [128, 128], BF16)
        k.op("dve", lambda e: e.memset(self.ones_bf[:], 1.0), [], [self.ones_bf])
        self.ones_f = k.sb("ones_f", [128, 128], F32)
        k.op("dve", lambda e: e.memset(self.ones_f[:], 1.0), [], [self.ones_f])
        self.eps_ada = k.sb("eps_ada", [128, 1], F32)
        k.op("dve", lambda e: e.memset(self.eps_ada[:], ADA_EPS), [], [self.eps_ada])
        self.eps_ln = k.sb("eps_ln", [128, 1], F32)
        k.op("dve", lambda e: e.memset(self.eps_ln[:], LN_EPS), [], [self.eps_ln])
        self.eps_5 = self.eps_ln
        mtmp = k.sb("mtmp", [128, 128], F32)
        self.maskP = k.sb("maskP_sb", [128, 128], BF16)
        self.maskN = k.sb("maskN_sb", [128, 128], BF16)
        k.dma("sp", mtmp[:], self.maskP_in.ap(), [self.maskP_in], [mtmp])
        k.copy("dve", self.maskP[:], mtmp[:], [mtmp], [self.maskP])
        k.dma("sp", mtmp[:], self.maskN_in.ap(), [self.maskN_in], [mtmp])
        k.copy("dve", self.maskN[:], mtmp[:], [mtmp], [self.maskN])

    def load_bc(self, dst, l, r, j, add_one):
        k = self.k
        NR = self.NR
        k.dma("sp", dst[:], self.modv[l * NR + r:l * NR + r + 1, j * D:(j + 1) * D].partition_broadcast(128),
              [self.modv], [dst])
        if add_one:
            k.ts("pool", dst[:], dst[:], 1.0, None, ALU.add, None, [dst], [dst])

    def ln_stats(self, ph_tiles, x_tile, eps_tile):
        k = self.k
        stats, mv, rstd = ph_tiles
        for c4 in range(4):
            k.op("dve", lambda e, c4=c4: e.bn_stats(stats[:, c4, :], x_tile[:, c4 * 512:(c4 + 1) * 512]),
                 [x_tile], [stats])
        k.op("dve", lambda e: e.bn_aggr(mv[:], stats[:]), [stats], [mv])
        k.act(rstd[:], mv[:, 1:2], AF.Sqrt, [mv, eps_tile], [rstd], bias=eps_tile[:], scale=1.0)
        k.op("dve", lambda e: e.reciprocal(rstd[:], rstd[:]), [rstd], [rstd])

    def phase_proj(self, l, b):
        cfg, k, ps = self.cfg, self.k, self.ps
        NB, L, C, NT = cfg.nb, cfg.L, cfg.C, cfg.NT
        xs, pT, identb = self.xs, self.pT, self.identb
        with self.phase(f"proj{l}_{b}") as ph:
            uT = ph.sb("uT", [128, KD, L], BF16)
            xt = [ph.sb(f"xt{i}", [128, D], F32) for i in range(2)]
            ub = [ph.sb(f"ub{i}", [128, D], BF16) for i in range(2)]
            bc = {nm: ph.sb(nm, [128, D], F32) for nm in ("sc", "sh", "scc", "shc")}
            st3 = (ph.sb("stats", [128, 4, 6], F32), ph.sb("mv", [128, 2], F32), ph.sb("rstd", [128, 1], F32))
            wst = [ph.sb(f"wst{i}", [128, KD * 128], F32) for i in range(2)]
            wbf = [ph.sb(f"wbf{i}", [128, KD, 128], BF16) for i in range(3)]
            ot = [ph.sb(f"ot{i}", [128, L], F32) for i in range(2)]
            self.load_bc(bc["scc"], l, NB, 1, True)
            self.load_bc(bc["shc"], l, NB, 0, False)
            self.load_bc(bc["sc"], l, b, 1, True)
            self.load_bc(bc["sh"], l, b, 0, False)
            for t in range(NT):
                x_t = xt[t % 2]
                u_t = ub[t % 2]
                k.dma("sp" if t % 2 == 0 else "sp", x_t[:], xs[b][t * 128:(t + 1) * 128, :], [xs[b]], [x_t])
                isctx = t * 128 < C
                sc_t, sh_t = (bc["scc"], bc["shc"]) if isctx else (bc["sc"], bc["sh"])
                self.ln_stats(st3, x_t, self.eps_ada)
                k.ts("dve", x_t[:], x_t[:], st3[1][:, 0:1], st3[2][:, 0:1], ALU.subtract, ALU.mult,
                     [x_t, st3[1], st3[2]], [x_t])
                k.tt("pool", x_t[:], x_t[:], sc_t[:], ALU.mult, [x_t, sc_t], [x_t])
                k.tt("dve", u_t[:], x_t[:], sh_t[:], ALU.add, [x_t, sh_t], [u_t])
                for g4 in range(4):
                    pst = ps[4 + (t * 4 + g4) % 4]
                    pv = pst.ap().bitcast(BF16)
                    for j in range(4):
                        kk = g4 * 4 + j
                        k.tr(pv[:, j * 128:(j + 1) * 128], u_t[:, kk * 128:(kk + 1) * 128], identb[:],
                             [u_t, identb], [pst])
                    k.copy(self.alt(), uT[:, g4 * 4:(g4 + 1) * 4, t * 128:(t + 1) * 128],
                           pv[:, 0:512].rearrange("p (j n) -> p j n", j=4), [pst], [uT])
            nblk = (L + 511) // 512
            for ct in range(NT_EXT):
                st_ = wst[ct % 2]
                wb = wbf[ct % 3]
                o_t = ot[ct % 2]
                r0 = (l * NT_EXT + ct) * 128
                k.dma("sp" if ct % 2 == 0 else "sp", st_[:], self.w_ext[r0:r0 + 128, :], [self.w_ext], [st_])
                k.copy(self.alt(("dve", "pool")), wb[:], st_[:].rearrange("p (k n) -> p k n", k=KD), [st_], [wb])
                for tb in range(nblk):
                    t0 = tb * 512
                    n = min(512, L - t0)
                    pst = ps[(ct * nblk + tb) % 4]
                    for kk in range(KD):
                        k.mm(pst[:, 0:n], wb[:, kk, :], uT[:, kk, t0:t0 + n], kk == 0, kk == KD - 1,
                             [wb, uT], [pst])
                    k.copy(self.alt(), o_t[:, t0:t0 + n], pst[:, 0:n], [pst], [o_t])
                k.dma("pool", pT[ct * 128:(ct + 1) * 128, :], o_t[:], [o_t], [pT.sub(ct)])

    def rope_tile(self, ph, dst_buf, dst_bf, row0, prow0, tmp_a, tmp_b, rc, rs):
        k, pT, L = self.k, self.pT, self.cfg.L
        ct, pt_ = row0 // 128, prow0 // 128
        k.dma("sp", tmp_a[:], pT[row0:row0 + 128, :], [pT.sub(ct)], [tmp_a])
        k.dma("sp", tmp_b[:], pT[prow0:prow0 + 128, :], [pT.sub(pt_)], [tmp_b])
        k.tt("pool", tmp_a[:], tmp_a[:], rc[:], ALU.mult, [tmp_a, rc], [tmp_a])
        k.tt("dve", tmp_b[:], tmp_b[:], rs[:], ALU.mult, [tmp_b, rs], [tmp_b])
        k.tt("dve", dst_bf, tmp_a[:], tmp_b[:], ALU.add, [tmp_a, tmp_b], [dst_buf])

    def to_tokmajor(self, ph, dst_tok, row0, tmp_a, tmp_bf):
        k, pT, ps, NT = self.k, self.pT, self.ps, self.cfg.NT
        ct = row0 // 128
        k.dma("sp", tmp_a[:], pT[row0:row0 + 128, :], [pT.sub(ct)], [tmp_a])
        k.copy("pool", tmp_bf[:], tmp_a[:], [tmp_a], [tmp_bf])
        for t0 in range(0, NT, 4):
            nt = min(4, NT - t0)
            pst = ps[4 + (t0 // 4) % 4]
            pv = pst.ap().bitcast(BF16)
            for j in range(nt):
                t = t0 + j
                k.tr(pv[:, j * 128:(j + 1) * 128], tmp_bf[:, t * 128:(t + 1) * 128], self.identb[:],
                     [tmp_bf, self.identb], [pst])
            k.copy(self.alt(), dst_tok[:, t0:t0 + nt, :],
                   pv[:, 0:nt * 128].rearrange("p (j n) -> p j n", j=nt), [pst], [dst_tok])

    def phase_diff(self, l, b):
        cfg, k, ps = self.cfg, self.k, self.ps
        L, C, S, NT = cfg.L, cfg.C, cfg.S, cfg.NT
        lam_init = 0.8 - 0.6 * math.exp(-0.3 * l)
        scale = 64 ** -0.5
        sec = EXT_SEC
        with self.phase(f"diff{l}_{b}") as ph:
            rc = ph.sb("rc", [128, L], F32)
            rs = ph.sb("rs", [128, L], F32)
            k.dma("sp", rc[:], self.ropec.ap(), [self.ropec], [rc])
            k.dma("sp", rs[:], self.ropes.ap(), [self.ropes], [rs])
            ta = ph.sb("ta", [128, L], F32)
            tb_ = ph.sb("tb", [128, L], F32)
            tbf = ph.sb("tbf", [128, L], BF16)
            qr = ph.sb("qr", [128, L], BF16)
            kr = ph.sb("kr", [128, L], BF16)
            vtok = ph.sb("vtok", [128, NT, 128], BF16)
            pt = [ph.sb(f"pt{i}", [128, 512], BF16) for i in range(2)]
            rl = ph.sb("rl", [128, 512], F32)
            o0 = ph.sb("o0", [128, 512], F32)
            o1 = ph.sb("o1", [128, 512], F32)
            sq = ph.sb("sq", [128, 512], F32)
            yo = ph.sb("yo", [128, 512], BF16)
            dl = ph.sb("dl", [128, 256], F32)
            pr = ph.sb("pr", [128, 128], F32)
            e2 = ph.sb("e2", [128, 2], F32)
            nlam = ph.sb("nlam", [128, 1], F32)
            gsc = ph.sb("gsc", [128, 1], F32)
            k.dma("sp", dl[:], self.diff_lam[l:l + 1, :].partition_broadcast(128), [self.diff_lam], [dl])
            k.tt("dve", pr[:].rearrange("p (a d) -> p a d", a=2),
                 dl[:].rearrange("p (a t d) -> p a t d", a=2, t=2)[:, :, 0, :],
                 dl[:].rearrange("p (a t d) -> p a t d", a=2, t=2)[:, :, 1, :], ALU.mult, [dl], [pr])
            k.op("dve", lambda e: e.tensor_reduce(e2[:], pr[:].rearrange("p (a d) -> p a d", a=2), AX.X, ALU.add),
                 [pr], [e2])
            k.act(e2[:], e2[:], AF.Exp, [e2], [e2])
            k.tt("dve", nlam[:], e2[:, 1:2], e2[:, 0:1], ALU.subtract, [e2], [nlam])
            k.ts("dve", nlam[:], nlam[:], -lam_init, None, ALU.add, None, [nlam], [nlam])
            k.dma("sp", gsc[:], self.diff_subln[l:l + 1, :].rearrange("o (p q) -> (o p) q", q=1), [self.diff_subln], [gsc])
            k.ts("dve", gsc[:], gsc[:], 1.0 - lam_init, None, ALU.mult, None, [gsc], [gsc])
            qblocks = [(0, C, list(range(C // 128)))]
            q0 = C
            while q0 < L:
                nq = min(512, L - q0)
                qblocks.append((q0, nq, list(range(NT))))
                q0 += nq
            for h in range(6):
                self.rope_tile(ph, qr, qr[:], sec["dq"][0] + h * 128, sec["dqp"][0] + h * 128, ta, tb_, rc, rs)
                self.rope_tile(ph, kr, kr[:], sec["dk"][0] + h * 128, sec["dkp"][0] + h * 128, ta, tb_, rc, rs)
                self.to_tokmajor(ph, vtok, sec["dv"][0] + h * 128, ta, tbf)
                for (q0, nq, kts) in qblocks:
                    for m in range(2):
                        pso, psl = ps[m], ps[2 + m]
                        for i, kt in enumerate(kts):
                            pss = ps[6 + (i % 2)]
                            p_t = pt[i % 2]
                            k.mm(pss[:, 0:nq], kr[m * 64:(m + 1) * 64, kt * 128:(kt + 1) * 128],
                                 qr[m * 64:(m + 1) * 64, q0:q0 + nq], True, True, [kr, qr], [pss])
                            k.act(p_t[:, 0:nq], pss[:, 0:nq], AF.Exp, [pss], [p_t], scale=scale)
                            k.mm(pso[:, 0:nq], vtok[:, kt, :], p_t[:, 0:nq], i == 0, i == len(kts) - 1,
                                 [vtok, p_t], [pso])
                            k.mm(psl[:, 0:nq], self.ones_bf[:], p_t[:, 0:nq], i == 0, i == len(kts) - 1,
                                 [self.ones_bf, p_t], [psl])
                    k.op("dve", lambda e: e.reciprocal(rl[:, 0:nq], ps[2][:, 0:nq]), [ps[2]], [rl])
                    k.tt("dve", o0[:, 0:nq], ps[0][:, 0:nq], rl[:, 0:nq], ALU.mult, [ps[0], rl], [o0])
                    k.op("dve", lambda e: e.reciprocal(rl[:, 0:nq], ps[3][:, 0:nq]), [ps[3]], [rl])
                    k.tt("dve", o1[:, 0:nq], ps[1][:, 0:nq], rl[:, 0:nq], ALU.mult, [ps[1], rl], [o1])
                    k.stt(o0[:, 0:nq], o1[:, 0:nq], nlam[:, 0:1], o0[:, 0:nq], ALU.mult, ALU.add, [o1, nlam, o0], [o0])
                    k.tt("pool", sq[:, 0:nq], o0[:, 0:nq], o0[:, 0:nq], ALU.mult, [o0], [sq])
                    k.mm(ps[6][:, 0:nq], self.ones_f[:], sq[:, 0:nq], True, True, [self.ones_f, sq], [ps[6]])
                    k.act(rl[:, 0:nq], ps[6][:, 0:nq], AF.Sqrt, [ps[6], self.eps_5], [rl], bias=self.eps_5[:], scale=1.0 / 128)
                    k.op("dve", lambda e: e.reciprocal(rl[:, 0:nq], rl[:, 0:nq]), [rl], [rl])
                    k.tt("dve", o0[:, 0:nq], o0[:, 0:nq], rl[:, 0:nq], ALU.mult, [o0, rl], [o0])
                    k.ts("dve", yo[:, 0:nq], o0[:, 0:nq], gsc[:, 0:1], None, ALU.mult, None, [o0, gsc], [yo])
                    k.dma("pool", self.yT[768 + h * 128:768 + (h + 1) * 128, q0:q0 + nq], yo[:, 0:nq], [yo],
                          [self.yT.sub(("b", h, q0))])

    def _mark_write(self, buf, srcs):
        k = self.k
        buf.last_w = ("dve", k.cnt["dve"])
        buf.readers = {}

    def phase_win(self, l, b):
        cfg, k, ps = self.cfg, self.k, self.ps
        L, C, S, NT = cfg.L, cfg.C, cfg.S, cfg.NT
        scale = 64 ** -0.5
        sec = EXT_SEC
        NCT = C // 128
        with self.phase(f"win{l}_{b}") as ph:
            rc = ph.sb("rc", [128, L], F32)
            rs = ph.sb("rs", [128, L], F32)
            k.dma("sp", rc[:], self.ropec.ap(), [self.ropec], [rc])
            k.dma("sp", rs[:], self.ropes.ap(), [self.ropes], [rs])
            ta = ph.sb("ta", [128, L], F32)
            tb_ = ph.sb("tb", [128, L], F32)
            tbf = ph.sb("tbf", [128, L], BF16)
            wqr = ph.sb("wqr", [128, 4, L], BF16)
            wkr = ph.sb("wkr", [128, L], BF16)
            wvtok = ph.sb("wvtok", [128, NT, 128], BF16)
            pt = [ph.sb(f"pt{i}", [128, 4, 128], BF16) for i in range(2)]
            den = ph.sb("den", [64, 4, 128], F32)
            yo = ph.sb("yo", [64, 4, 128], BF16)
            es = ph.sb("es", [64, 8], F32)
            k.dma("sp", es[:], self.win_sink[l:l + 1, :].partition_broadcast(64), [self.win_sink], [es])
            k.act(es[:], es[:], AF.Exp, [es], [es])
            for g in range(4):
                self.rope_tile(ph, wqr, wqr[:, g, :], sec["wq"][0] + g * 128, sec["wqp"][0] + g * 128, ta, tb_, rc, rs)
            self.rope_tile(ph, wkr, wkr[:], sec["wk"][0], sec["wkp"][0], ta, tb_, rc, rs)
            self.to_tokmajor(ph, wvtok, sec["wv"][0], ta, tbf)
            for hkv in range(2):
                pb = slice(hkv * 64, (hkv + 1) * 64)
                for qi in range(NT):
                    if qi < NCT:
                        kts = [(kt, None) for kt in range(NCT)]
                    else:
                        j = qi - NCT
                        kts = [(kt, None) for kt in range(NCT)]
                        if j - 1 >= 0:
                            kts.append((qi - 1, self.maskP))
                        kts.append((qi, None))
                        if j + 1 < S // 128:
                            kts.append((qi + 1, self.maskN))
                    pso, psl = ps[0 + (qi % 2)], ps[2 + (qi % 2)]
                    for i, (kt, msk) in enumerate(kts):
                        pss = ps[6 + (i % 2)]
                        p_t = pt[i % 2]
                        k.mm(pss[:, :].rearrange("p (g n) -> p g n", g=4), wkr[pb, kt * 128:(kt + 1) * 128],
                             wqr[pb, :, qi * 128:(qi + 1) * 128], True, True, [wkr, wqr], [pss])
                        k.act(p_t[:].rearrange("p g n -> p (g n)"), pss[:, :], AF.Exp, [pss], [p_t], scale=scale)
                        if msk is not None:
                            k.tt("pool", p_t[:], p_t[:], msk[:].rearrange("p (o n) -> p o n", o=1).broadcast_to([128, 4, 128]),
                                 ALU.mult, [p_t, msk], [p_t])
                        k.mm(pso[0:64, :], wvtok[:, kt, pb], p_t[:].rearrange("p g n -> p (g n)"), i == 0,
                             i == len(kts) - 1, [wvtok, p_t], [pso])
                        k.mm(psl[0:64, :], self.ones_bf[:, 0:64], p_t[:].rearrange("p g n -> p (g n)"), i == 0,
                             i == len(kts) - 1, [self.ones_bf, p_t], [psl])
                    k.tt("dve", den[:], psl[0:64, :].rearrange("p (g n) -> p g n", g=4),
                         es[:, hkv * 4:(hkv + 1) * 4].rearrange("p (g o) -> p g o", o=1).broadcast_to([64, 4, 128]),
                         ALU.add, [psl, es], [den])
                    k.op("dve", lambda e: e.reciprocal(den[:], den[:]), [den], [den])
                    k.tt("dve", yo[:], pso[0:64, :].rearrange("p (g n) -> p g n", g=4), den[:], ALU.mult,
                         [pso, den], [yo])
                    r0 = 1536 + hkv * 256
                    k.dma("pool", self.yT[r0:r0 + 256, qi * 128:(qi + 1) * 128].rearrange("(g d) n -> d g n", g=4),
                          yo[:], [yo], [self.yT.sub(("c", hkv, qi))])

    def decl_rwkv(self):
        cfg, k = self.cfg, self.k
        DEP, L = cfg.depth, cfg.L
        self.rwvec = self.inp("rwvec", [DEP * 128, 96])
        self.w_up = self.inp("w_up", [DEP * 128, 768])
        self.a_up = self.inp("a_up", [DEP * 128, 768])
        self.g_up = self.inp("g_up", [DEP * 128, 768])
        self.maskA_in = self.inp("maskA", [2 * 128, 512])
        self.maskB_in = self.inp("maskB", [2 * 128, 128])
        self.ident2_in = self.inp("ident2", [128, 128])
        self.bones_in = self.inp("bones", [128, 128])
        self.resetm_in = self.inp("resetm", [128, L])
        for nm in ("fr", "fv", "fg", "fkk", "fbv", "flw0", "flw1", "fkd0", "fkd1", "fbd0", "fbd1"):
            setattr(self, nm, self.scratch(nm, [768, L]))
        self.bones = k.sb("bones_sb", [128, 128], F32)
        k.dma("sp", self.bones[:], self.bones_in.ap(), [self.bones_in], [self.bones])
        self.eps_gn = k.sb("eps_gn", [128, 1], F32)
        k.op("dve", lambda e: e.memset(self.eps_gn[:], 64e-5), [], [self.eps_gn])

    def phase_rwkv_feat(self, l, b):
        cfg, k, ps, pT = self.cfg, self.k, self.ps, self.pT
        L, C, S = cfg.L, cfg.C, cfg.S
        segs = [(0, C), (C, L)]
        nblk = (L + 511) // 512
        with self.phase(f"rwf{l}_{b}") as ph:
            vec = ph.sb("vec", [128, 96], F32)
            k.dma("sp", vec[:], self.rwvec[l * 128:(l + 1) * 128, :], [self.rwvec], [vec])
            cm = ph.sb("cm", [128, 21], F32)
            k.tt("dve", cm[:], vec[:, 0:21], vec[:, 21:42], ALU.add, [vec], [cm])
            k.ts("dve", cm[:], cm[:], -1.0, 1.0, ALU.mult, ALU.add, [cm], [cm])
            omka = ph.sb("omka", [128, 6], F32)
            k.ts("dve", omka[:], vec[:, 72:78], -1.0, 1.0, ALU.mult, ALU.add, [vec], [omka])
            wup = ph.sb("wup", [128, 768], F32)
            aup = ph.sb("aup", [128, 768], F32)
            gup = ph.sb("gup", [128, 768], F32)
            k.dma("sp", wup[:], self.w_up[l * 128:(l + 1) * 128, :], [self.w_up], [wup])
            k.dma("sp", aup[:], self.a_up[l * 128:(l + 1) * 128, :], [self.a_up], [aup])
            k.dma("sp", gup[:], self.g_up[l * 128:(l + 1) * 128, :], [self.g_up], [gup])
            pin = [ph.sb(f"pin{i}", [128, L], F32) for i in range(2)]
            twd = ph.sb("twd", [128, L], F32)
            ads = ph.sb("ads", [128, L], F32)
            sgd = ph.sb("sgd", [128, L], F32)
            rS = ph.sb("rS", [128, L], F32)
            vS = ph.sb("vS", [128, L], F32)
            kS = ph.sb("kS", [128, L], F32)
            kk_ = ph.sb("kk", [128, L], F32)
            t1 = ph.sb("t1", [128, L], F32)
            t2 = ph.sb("t2", [128, L], F32)
            aT = [ph.sb(f"aT{d}", [128, L], F32) for d in range(2)]
            kdT = [ph.sb(f"kdT{d}", [128, L], F32) for d in range(2)]
            self._pin_i = 0

            def shifted(tile_idx, dst):
                p_in = pin[self._pin_i % 2]
                self._pin_i += 1
                k.dma("sp" if self._pin_i % 2 else "sp", p_in[:], pT[tile_idx * 128:(tile_idx + 1) * 128, :],
                      [pT.sub(tile_idx)], [p_in])
                k.ts("dve", dst[:], p_in[:], cm[:, tile_idx:tile_idx + 1], None, ALU.mult, None, [p_in, cm], [dst])
                for (a, e) in segs:
                    k.stt(dst[:, a + 1:e], p_in[:, a:e - 1], vec[:, tile_idx:tile_idx + 1], dst[:, a + 1:e],
                          ALU.mult, ALU.add, [p_in, vec, dst], [dst])
                    k.stt(dst[:, a:e - 1], p_in[:, a + 1:e], vec[:, 21 + tile_idx:22 + tile_idx], dst[:, a:e - 1],
                          ALU.mult, ALU.add, [p_in, vec, dst], [dst])

            shifted(18, twd)
            k.act(twd[:], twd[:], AF.Tanh, [twd], [twd])
            shifted(19, ads)
            shifted(20, sgd)
            k.act(sgd[:], sgd[:], AF.Sigmoid, [sgd], [sgd])
            flw = [self.flw0, self.flw1]
            fkd = [self.fkd0, self.fkd1]
            fbd = [self.fbd0, self.fbd1]
            NEG_EH = -math.exp(-0.5)
            for c in range(6):
                cs = slice(c * 128, (c + 1) * 128)
                for d in range(2):
                    pb = slice(d * 64, (d + 1) * 64)
                    for tb in range(nblk):
                        t0 = tb * 512
                        n = min(512, L - t0)
                        pst = ps[(tb) % 4]
                        k.mm(pst[:, 0:n], wup[pb, cs], twd[pb, t0:t0 + n], True, True, [wup, twd], [pst])
                        k.act(t1[:, t0:t0 + n], pst[:, 0:n], AF.Sigmoid, [pst, vec], [t1],
                              bias=vec[:, 42 + d * 6 + c:43 + d * 6 + c], scale=1.0)
                        pst2 = ps[4 + (tb) % 4]
                        k.mm(pst2[:, 0:n], aup[pb, cs], ads[pb, t0:t0 + n], True, True, [aup, ads], [pst2])
                        k.act(aT[d][:, t0:t0 + n], pst2[:, 0:n], AF.Sigmoid, [pst2, vec], [aT[d]],
                              bias=vec[:, 54 + d * 6 + c:55 + d * 6 + c], scale=1.0)
                    k.ts("pool", t1[:], t1[:], NEG_EH, None, ALU.mult, None, [t1], [t1])
                    k.dma("pool", flw[d][cs, :], t1[:], [t1], [flw[d].sub(c)])
                for tb in range(nblk):
                    t0 = tb * 512
                    n = min(512, L - t0)
                    pst = ps[tb % 4]
                    k.mm(pst[:, 0:n], gup[:, cs], sgd[:, t0:t0 + n], True, True, [gup, sgd], [pst])
                    k.copy("act", t2[:, t0:t0 + n], pst[:, 0:n], [pst], [t2])
                k.dma("pool", self.fg[cs, :], t2[:], [t2], [self.fg.sub(c)])
                shifted(c, rS)
                k.dma("pool", self.fr[cs, :], rS[:], [rS], [self.fr.sub(c)])
                shifted(12 + c, vS)
                k.dma("pool", self.fv[cs, :], vS[:], [vS], [self.fv.sub(c)])
                shifted(6 + c, kS)
                k.ts("dve", kk_[:], kS[:], vec[:, 66 + c:67 + c], None, ALU.mult, None, [kS, vec], [kk_])
                k.tt("pool", t1[:], kk_[:], kk_[:], ALU.mult, [kk_], [t1])
                for tb in range(nblk):
                    t0 = tb * 512
                    n = min(512, L - t0)
                    pst = ps[tb % 4]
                    k.mm(pst[:, 0:n], self.bones[:], t1[:, t0:t0 + n], True, True, [self.bones, t1], [pst])
                    k.act(t2[:, t0:t0 + n], pst[:, 0:n], AF.Sqrt, [pst], [t2])
                k.ts("dve", t2[:], t2[:], 1e-12, None, ALU.max, None, [t2], [t2])
                k.op("dve", lambda e: e.reciprocal(t2[:], t2[:]), [t2], [t2])
                k.tt("dve", kk_[:], kk_[:], t2[:], ALU.mult, [kk_, t2], [kk_])
                k.dma("pool", self.fkk[cs, :], kk_[:], [kk_], [self.fkk.sub(c)])
                for d in range(2):
                    k.ts("dve", t1[:], aT[d][:], vec[:, 72 + c:73 + c], omka[:, c:c + 1], ALU.mult, ALU.add,
                         [aT[d], vec, omka], [t1])
                    k.tt("dve", kdT[d][:], t1[:], kS[:], ALU.mult, [t1, kS], [kdT[d]])
                    k.dma("pool", fkd[d][cs, :], kdT[d][:], [kdT[d]], [fkd[d].sub(c)])
                    k.tt("pool", t2[:], kk_[:], aT[d][:], ALU.mult, [kk_, aT[d]], [t2])
                    k.dma("pool", fbd[d][cs, :], t2[:], [t2], [fbd[d].sub(c)])
                k.tt("dve", t1[:], kdT[0][:], kdT[1][:], ALU.add, [kdT[0], kdT[1]], [t1])
                k.tt("dve", t1[:], t1[:], rS[:], ALU.mult, [t1, rS], [t1])
                k.ts("dve", t1[:], t1[:], vec[:, 78 + c:79 + c], None, ALU.mult, None, [t1, vec], [t1])
                for tb in range(nblk):
                    t0 = tb * 512
                    n = min(512, L - t0)
                    pst = ps[tb % 4]
                    k.mm(pst[:, 0:n], self.bones[:], t1[:, t0:t0 + n], True, True, [self.bones, t1], [pst])
                    k.tt("dve", t2[:, t0:t0 + n], pst[:, 0:n], vS[:, t0:t0 + n], ALU.mult, [pst, vS], [t2])
                k.dma("pool", self.fbv[cs, :], t2[:], [t2], [self.fbv.sub(c)])

    def phase_rwkv_scan(self, l, b):
        cfg, k, ps = self.cfg, self.k, self.ps
        L, C, S, NT = cfg.L, cfg.C, cfg.S, cfg.NT
        NCH = L // 64
        NCC = C // 64
        nblk = (L + 511) // 512
        flw = [self.flw0, self.flw1]
        fkd = [self.fkd0, self.fkd1]
        fbd = [self.fbd0, self.fbd1]
        with self.phase(f"rws{l}_{b}") as ph:
            vec = ph.sb("vec", [128, 96], F32)
            k.dma("sp", vec[:], self.rwvec[l * 128:(l + 1) * 128, :], [self.rwvec], [vec])
            maskA = [ph.sb(f"maskA{d}", [64, 512], F32) for d in range(2)]
            maskB = [ph.sb(f"maskB{d}", [64, 128], F32) for d in range(2)]
            for d in range(2):
                k.dma("sp", maskA[d][:], self.maskA_in[d * 128:d * 128 + 64, :], [self.maskA_in], [maskA[d]])
                k.dma("sp", maskB[d][:], self.maskB_in[d * 128:d * 128 + 64, :], [self.maskB_in], [maskB[d]])
            ident2 = ph.sb("ident2", [64, 128], F32)
            k.dma("sp", ident2[:], self.ident2_in[0:64, :], [self.ident2_in], [ident2])
            resetm = ph.sb("resetm", [128, L], F32)
            k.dma("sp", resetm[:], self.resetm_in.ap(), [self.resetm_in], [resetm])
            big1 = ph.sb("big1", [128, 2 * L], F32)
            big2 = ph.sb("big2", [128, 2 * L], F32)
            big3 = ph.sb("big3", [128, L], F32)
            katBD_t = ph.sb("katBD", [128, 2 * L], BF16)
            btBD_t = ph.sb("btBD", [128, 2 * L], BF16)
            rtBD_t = ph.sb("rtBD", [128, 2 * L], BF16)
            E1 = ph.sb("E1", [128, L], F32)
            rt32 = ph.sb("rt32", [128, L], F32)
            kat32 = ph.sb("kat32", [128, L], F32)
            kt32 = ph.sb("kt32", [128, L], F32)
            bt32 = ph.sb("bt32", [128, L], F32)
            rt = ph.sb("rt", [128, L], BF16)
            kat = ph.sb("kat", [128, L], BF16)
            kt_ = ph.sb("kt", [128, L], BF16)
            bt = ph.sb("bt", [128, L], BF16)
            Ktok = ph.sb("Ktok", [64, NCH, 128], BF16)
            Btok = ph.sb("Btok", [64, NCH, 128], BF16)
            Vtok = ph.sb("Vtok", [64, NCH, 128], BF16)
            yacc = ph.sb("yacc", [128, L], F32)
            Asb = ph.sb("Asb", [64, 8, 64], BF16)
            Bsb = ph.sb("Bsb", [64, 2, 64], BF16)
            Psb = ph.sb("Psb", [64, 4, 64], BF16)
            Rsb = ph.sb("Rsb", [64, 2, 64], BF16)
            Asb2 = ph.sb("Asb2", [64, 8, 64], BF16)
            Bsb2 = ph.sb("Bsb2", [64, 2, 64], BF16)
            Rsb2 = ph.sb("Rsb2", [64, 2, 64], BF16)
            Wsb = ph.sb("Wsb", [64, 2, 64], BF16)
            Usb = ph.sb("Usb", [64, 2, 64], BF16)
            H = ph.sb("H", [128, 64], F32)
            Hbd = ph.sb("Hbd", [128, 128], BF16)
            Hbd32 = ph.sb("Hbd32", [128, 128], F32)
            Htmp = ph.sb("Htmp", [128, 128], F32)
            yo = ph.sb("yo", [128, L], BF16)
            lw, cl = big1[:, 0:L], big1[:, L:2 * L]
            E2, t1 = big2[:, 0:L], big2[:, L:2 * L]
            vv = big3[:, 0:L]

            def tok_major(dst, src_buf, src_ap, neg=False, bf=True):
                for c0 in range(0, NCH, 4):
                    n4 = min(4, NCH - c0)
                    pst = ps[4 + (c0 // 4) % 2]
                    pv = pst.ap().bitcast(BF16) if bf else pst.ap()
                    idn = self.identb if bf else self.identf
                    for j in range(n4):
                        ch = c0 + j
                        k.tr(pv[0:64, j * 128:(j + 1) * 128], src_ap[:, ch * 64:(ch + 1) * 64], idn[:],
                             [src_buf, idn], [pst])
                    if neg:
                        k.ts("dve", dst[:, c0:c0 + n4, :], pv[0:64, 0:n4 * 128].rearrange("p (j n) -> p j n", j=n4),
                             -1.0, None, ALU.mult, None, [pst], [dst])
                    else:
                        k.copy(self.alt(), dst[:, c0:c0 + n4, :],
                               pv[0:64, 0:n4 * 128].rearrange("p (j n) -> p j n", j=n4), [pst], [dst])

            def make_bd(dst_big, src):
                d4 = dst_big[:].rearrange("p (c h n) -> p c h n", h=2, n=64)
                s3 = src[:].rearrange("p (c n) -> p c n", n=64)
                k.op("pool", lambda e: e.memset(dst_big[:], 0.0), [], [dst_big])
                k.copy("dve", d4[0:64, :, 0, :], s3[0:64], [src], [dst_big])
                k.copy("act", d4[64:128, :, 1, :], s3[64:128], [src], [dst_big])

            for c in range(6):
                cs = slice(c * 128, (c + 1) * 128)
                k.dma("sp", vv, self.fv[cs, :], [self.fv.sub(c)], [big3])
                tok_major(Vtok, big3, vv, bf=False)
                for d in range(2):
                    k.dma("sp", lw, flw[d][cs, :], [flw[d].sub(c)], [big1])
                    k.dma("sp", rt32[:], self.fr[cs, :], [self.fr.sub(c)], [rt32])
                    k.dma("sp", kat32[:], self.fkk[cs, :], [self.fkk.sub(c)], [kat32])
                    k.dma("sp", kt32[:], fkd[d][cs, :], [fkd[d].sub(c)], [kt32])
                    k.dma("sp", bt32[:], fbd[d][cs, :], [fbd[d].sub(c)], [bt32])
                    k.op("dve", lambda e: e.tensor_tensor_scan(cl, resetm[:], lw, 0.0, ALU.mult, ALU.add),
                         [resetm, big1], [big1])
                    if d == 1:
                        cl3 = cl.rearrange("p (c n) -> p c n", n=64)
                        k.tt("dve", t1.rearrange("p (c n) -> p c n", n=64), cl3[:, :, 63:64].broadcast_to([128, NCH, 64]),
                             cl3, ALU.subtract, [big1], [big2])
                        k.tt("dve", cl, t1, lw, ALU.add, [big2, big1], [big1])
                    k.act(E1[:], cl, AF.Exp, [big1], [E1])
                    k.act(E2, cl, AF.Exp, [big1], [big2], scale=-1.0)
                    k.tt("dve", t1, cl, lw, ALU.subtract, [big1], [big2])
                    k.act(t1, t1, AF.Exp, [big2], [big2])
                    k.tt("dve", rt[:], rt32[:], E1[:], ALU.mult, [rt32, E1], [rt])
                    k.tt("pool", kat[:], kat32[:], t1, ALU.mult, [kat32, big2], [kat])
                    k.tt("dve", kt_[:], kt32[:], E2, ALU.mult, [kt32, big2], [kt_])
                    k.tt("pool", bt[:], bt32[:], E2, ALU.mult, [bt32, big2], [bt])
                    tok_major(Ktok, kt_, kt_[:])
                    tok_major(Btok, bt, bt[:], neg=True)
                    make_bd(katBD_t, kat)
                    make_bd(btBD_t, bt)
                    make_bd(rtBD_t, rt)
                    katBD = katBD_t[:].rearrange("p (c m) -> p c m", m=128)
                    btBD = btBD_t[:].rearrange("p (c m) -> p c m", m=128)
                    rtBD = rtBD_t[:].rearrange("p (c m) -> p c m", m=128)
                    k.op("dve", lambda e: e.memset(H[:], 0.0), [], [H])
                    k.op("dve", lambda e: e.memset(Hbd[:], 0.0), [], [Hbd])
                    k.op("dve", lambda e: e.memset(Hbd32[:], 0.0), [], [Hbd32])
                    if d == 0:
                        order = list(range(NCH))
                    else:
                        order = list(range(NCC - 1, -1, -1)) + list(range(NCH - 1, NCC - 1, -1))
                    psA, psB, psP, psR, psW, psY, psH = ps[0], ps[1], ps[2], ps[3], ps[6], ps[7], ps[5]
                    flat = lambda t: t[:].rearrange("p a n -> p (a n)")

                    def pre_gen(ch, A_, B_, R_):
                        cc = slice(ch * 64, (ch + 1) * 64)
                        k.mm(psA[0:64, 0:128], bt[:, cc], katBD[:, ch, :], True, True, [bt, katBD_t], [psA])
                        k.mm(psA[0:64, 128:256], kat[:, cc], btBD[:, ch, :], True, True, [kat, btBD_t], [psA])
                        k.mm(psA[0:64, 256:384], kt_[:, cc], katBD[:, ch, :], True, True, [kt_, katBD_t], [psA])
                        k.mm(psA[0:64, 384:512], kt_[:, cc], rtBD[:, ch, :], True, True, [kt_, rtBD_t], [psA])
                        k.mm(psB[0:64, 0:128], bt[:, cc], rtBD[:, ch, :], True, True, [bt, rtBD_t], [psB])
                        yield
                        k.tt("dve", flat(A_), psA[0:64, :], maskA[d][:], ALU.mult, [psA, maskA[d]], [A_])
                        k.tt("dve", flat(B_), psB[0:64, 0:128], maskB[d][:], ALU.mult, [psB, maskB[d]], [B_])
                        k.tt("dve", flat(R_), ident2[:], A_[:, 0:2, :].rearrange("p a n -> p (a n)"), ALU.subtract, [ident2, A_], [R_])
                        yield
                        Pc, PTc = (A_, 0), (A_, 2)
                        for lev in range(5):
                            last = lev == 4
                            for hh in range(2):
                                Pm = Pc[0][:, Pc[1] + hh, :]
                                PTm = PTc[0][:, PTc[1] + hh, :]
                                if not last:
                                    k.mm(psP[0:64, hh * 64:(hh + 1) * 64], PTm, Pm, True, True, [Pc[0]], [psP])
                                k.mm(psP[0:64, (2 + hh) * 64:(3 + hh) * 64], Pm, PTm, True, True, [Pc[0]], [psP])
                            yield
                            if not last:
                                k.copy("act", flat(Psb), psP[0:64, 0:256], [psP], [Psb])
                            else:
                                k.copy("act", Psb[:, 2:4, :].rearrange("p a n -> p (a n)"), psP[0:64, 128:256], [psP], [Psb])
                            yield
                            Pc, PTc = (Psb, 0), (Psb, 2)
                            for hh in range(2):
                                k.mm(psR[0:64, hh * 64:(hh + 1) * 64], Psb[:, 2 + hh, :], R_[:, hh, :], True, True, [Psb, R_], [psR])
                            yield
                            k.tt("dve", flat(R_), flat(R_), psR[0:64, 0:128], ALU.add, [R_, psR], [R_])
                            yield

                    def ser_gen(ch, A_, B_, R_):
                        cc = slice(ch * 64, (ch + 1) * 64)
                        k.mm(psW[0:64, 0:128], kat[:, cc], Hbd[:], True, False, [kat, Hbd], [psW])
                        for hh in range(2):
                            hp = slice(hh * 64, (hh + 1) * 64)
                            k.mm(psW[0:64, hh * 64:(hh + 1) * 64], A_[:, 4 + hh, :], Vtok[:, ch, hp], False, hh == 1, [A_, Vtok], [psW])
                        yield
                        k.copy("act", flat(Wsb), psW[0:64, 0:128], [psW], [Wsb])
                        yield
                        for hh in range(2):
                            k.mm(psW[0:64, (2 + hh) * 64:(3 + hh) * 64], R_[:, hh, :], Wsb[:, hh, :], True, True, [R_, Wsb], [psW])
                        yield
                        k.copy("act", flat(Usb), psW[0:64, 128:256], [psW], [Usb])
                        yield
                        ycol = slice((ch % 4) * 128, (ch % 4 + 1) * 128)
                        k.mm(psY[:, ycol], Hbd[:], rtBD[:, ch, :], True, False, [Hbd, rtBD_t], [psY])
                        k.mm(psY[:, ycol], Vtok[:, ch, :], A_[:, 6:8, :].rearrange("p a n -> p (a n)"), False, False, [Vtok, A_], [psY])
                        k.mm(psY[:, ycol], flat(Usb), flat(B_), False, True, [Usb, B_], [psY])
                        k.mm(psH[:, 0:128], Ktok[:, ch, :], Vtok[:, ch, :], True, False, [Ktok, Vtok], [psH])
                        k.mm(psH[:, 0:128], Btok[:, ch, :], flat(Usb), False, True, [Btok, Usb], [psH])
                        yield
                        gcol = ch * 64 + (63 if d == 0 else 0)
                        k.tt("dve", Htmp[:], Hbd32[:], psH[:, 0:128], ALU.add, [Hbd32, psH], [Htmp])
                        k.stt(Hbd32[:], Htmp[:], E1[:, gcol:gcol + 1], self.bones[:], ALU.mult, ALU.mult, [Htmp, E1, self.bones], [Hbd32])
                        k.copy("pool", Hbd[:], Hbd32[:], [Hbd32], [Hbd])
                        for hh in range(2):
                            hp = slice(hh * 64, (hh + 1) * 64)
                            ysrc = psY[hp, (ch % 4) * 128 + hh * 64:(ch % 4) * 128 + (hh + 1) * 64]
                            if d == 0:
                                k.copy("act", yacc[hp, cc], ysrc, [psY], [yacc])
                            else:
                                k.tt("pool" if False else "dve", yacc[hp, cc], yacc[hp, cc], ysrc, ALU.add, [yacc, psY], [yacc])
                        yield

                    bufs = [(Asb, Bsb, Rsb), (Asb2, Bsb2, Rsb2)]
                    for _ in pre_gen(order[0], *bufs[0]):
                        pass
                    for i, ch in enumerate(order):
                        gens = [ser_gen(ch, *bufs[i % 2])]
                        if i + 1 < len(order):
                            gens.append(pre_gen(order[i + 1], *bufs[(i + 1) % 2]))
                        while gens:
                            for g_ in list(gens):
                                try:
                                    next(g_)
                                except StopIteration:
                                    gens.remove(g_)
                gT, bvT = big1[:, 0:L], big1[:, L:2 * L]
                E2r, t1r = big2[:, 0:L], big2[:, L:2 * L]
                k.dma("sp", gT, self.fg[cs, :], [self.fg.sub(c)], [big1])
                k.dma("sp", bvT, self.fbv[cs, :], [self.fbv.sub(c)], [big1])
                for tb in range(nblk):
                    t0 = tb * 512
                    n = min(512, L - t0)
                    sl = slice(t0, t0 + n)
                    k.mm(ps[0][:, 0:n], self.bones[:], yacc[:, sl], True, True, [self.bones, yacc], [ps[0]])
                    k.stt(E1[:, sl], ps[0][:, 0:n], -1.0 / 64, yacc[:, sl], ALU.mult, ALU.add, [ps[0], yacc], [E1])
                    k.tt("pool", E2r[:, sl], E1[:, sl], E1[:, sl], ALU.mult, [E1], [big2])
                    k.mm(ps[1][:, 0:n], self.bones[:], E2r[:, sl], True, True, [self.bones, big2], [ps[1]])
                    k.act(t1r[:, sl], ps[1][:, 0:n], AF.Sqrt, [ps[1], self.eps_gn], [big2], bias=self.eps_gn[:], scale=1.0 / 64)
                k.op("dve", lambda e: e.reciprocal(t1r, t1r), [big2], [big2])
                k.tt("dve", E1[:], E1[:], t1r, ALU.mult, [E1, big2], [E1])
                k.ts("dve", E1[:], E1[:], vec[:, 84 + c:85 + c], vec[:, 90 + c:91 + c], ALU.mult, ALU.add, [E1, vec], [E1])
                k.tt("dve", E1[:], E1[:], bvT, ALU.add, [E1, big1], [E1])
                k.tt("dve", yo[:], E1[:], gT, ALU.mult, [E1, big1], [yo])
                k.dma("pool", self.yT[cs, :], yo[:], [yo], [self.yT.sub(("a", c))])

    def decl_moe(self):
        cfg, k = self.cfg, self.k
        DEP, L, NB = cfg.depth, cfg.L, cfg.nb
        self.CAP = cfg.cap
        self.NSLOT = 32 * self.CAP
        self.T = NB * L
        self.NTT = NB * cfg.NT
        self.w_branch = self.inp("w_branch", [DEP * D, D])
        self.w_out = self.inp("w_out", [DEP * D, D])
        self.ln_g = self.inp("ln_g", [DEP * 2, D])
        self.ln_b = self.inp("ln_b", [DEP * 2, D])
        self.w_r = self.inp("w_r", [DEP * 128, KD * 36])
        self.b_r = self.inp("b_r", [DEP, 36])
        self.w1r = self.inp("w1r", [DEP * 32 * 128, 8192])
        self.w3r = self.inp("w3r", [DEP * 32 * 128, 8192])
        self.w2r = self.inp("w2r", [DEP * 32 * 128, 8192])
        self.triu_in = self.inp("triu", [128, 128])
        self.ecap_in = self.inp("ecap", [128, 32])
        self.cnt_out = self.scratch("cnt", [DEP * 128, 32])
        self.zT = self.scratch("zT", [D, L], BF16)
        self.xslots = self.scratch("xslots", [self.NSLOT, D], BF16)
        self.yslots = [self.scratch(f"yslots{i}", [self.NSLOT // 2, D], F32) for i in range(2)]
        self.breg_h = self.nc.gpsimd.to_reg(self.NSLOT // 2 - 1)
        self.slotB_sb = k.sb("slotB_sb", [128, 2 * (cfg.nb * cfg.NT)], I32)
        self.triu = k.sb("triu_sb", [128, 128], F32)
        k.dma("sp", self.triu[:], self.triu_in.ap(), [self.triu_in], [self.triu])
        self.ecap = k.sb("ecap_sb", [128, 32], F32)
        k.dma("sp", self.ecap[:], self.ecap_in.ap(), [self.ecap_in], [self.ecap])
        self.breg = self.nc.gpsimd.to_reg(self.NSLOT - 1)
        self.off = k.sb("moe_off", [128, 32], F32)
        self.slot_sb = k.sb("slot_sb", [128, 2 * self.NTT], I32)
        self.gate_sb = k.sb("gate_sb", [128, 2 * self.NTT], F32)

    def load_w_bf(self, ph, wb, w_dram, row0, stage, nchunks=16):
        k = self.k
        for kc in range(nchunks):
            st = stage[kc % 2]
            k.dma("sp" if kc % 2 == 0 else "sp", st[:], w_dram[row0 + kc * 128:row0 + (kc + 1) * 128, :], [w_dram], [st])
            k.copy(self.alt(("dve", "pool")), wb[:, kc, :], st[:], [st], [wb])

    def phase_merge(self, l, b):
        cfg, k, ps, pT = self.cfg, self.k, self.ps, self.pT
        L = cfg.L
        g0 = EXT_SEC["gate"][0]
        with self.phase(f"mrg{l}_{b}") as ph:
            wb = ph.sb("wb", [128, KD, D], BF16)
            with self.phase(f"ldwb{l}_{b}") as p2:
                stage = [p2.sb(f"stg{i}", [128, D], F32) for i in range(2)]
                self.load_w_bf(p2, wb, self.w_branch, l * D, stage)
            yblk = ph.sb("yblk", [128, KD, 512], BF16)
            zblk = ph.sb("zblk", [128, KD, 512], BF16)
            gt = [ph.sb(f"gt{i}", [128, 3, 512], F32) for i in range(2)]
            za = ph.sb("za", [128, 512], F32)
            zb = ph.sb("zb", [128, 512], F32)
            t0 = 0
            while t0 < L:
                n = min(512, L - t0)
                k.dma("sp", yblk[:, :, 0:n], self.yT[:, t0:t0 + n].rearrange("(k p) n -> p k n", p=128), [self.yT], [yblk])
                for nt in range(16):
                    g = gt[nt % 2]
                    for br in range(3):
                        r0 = g0 + br * D + nt * 128
                        k.dma("sp", g[:, br, 0:n], pT[r0:r0 + 128, t0:t0 + n], [pT.sub(r0 // 128)], [g])
                    k.act(g[:, :, 0:n], g[:, :, 0:n], AF.Sigmoid, [g], [g])
                    pa, pb_, pc = ps[(nt % 2) * 3 + 0], ps[(nt % 2) * 3 + 1], ps[(nt % 2) * 3 + 2]
                    for (pst, k0, k1) in ((pa, 0, 6), (pb_, 6, 12), (pc, 12, 16)):
                        for kc in range(k0, k1):
                            k.mm(pst[:, 0:n], wb[:, kc, nt * 128:(nt + 1) * 128], yblk[:, kc, 0:n], kc == k0, kc == k1 - 1,
                                 [wb, yblk], [pst])
                    k.tt("dve", za[:, 0:n], pa[:, 0:n], g[:, 0, 0:n], ALU.mult, [pa, g], [za])
                    k.tt("dve", zb[:, 0:n], pb_[:, 0:n], g[:, 1, 0:n], ALU.mult, [pb_, g], [zb])
                    k.tt("pool", za[:, 0:n], za[:, 0:n], zb[:, 0:n], ALU.add, [za, zb], [za])
                    k.tt("dve", zb[:, 0:n], pc[:, 0:n], g[:, 2, 0:n], ALU.mult, [pc, g], [zb])
                    k.tt("pool", zblk[:, nt, 0:n], za[:, 0:n], zb[:, 0:n], ALU.add, [za, zb], [zblk])
                k.dma("pool", self.zT[:, t0:t0 + n].rearrange("(k p) n -> p k n", p=128), zblk[:, :, 0:n], [zblk], [self.zT])
                t0 += n

    def ln_affine(self, st3, x_t, eps_tile, g_bc, b_bc):
        k = self.k
        self.ln_stats(st3, x_t, eps_tile)
        k.ts("dve", x_t[:], x_t[:], st3[1][:, 0:1], st3[2][:, 0:1], ALU.subtract, ALU.mult, [x_t, st3[1], st3[2]], [x_t])
        k.tt("pool", x_t[:], x_t[:], g_bc[:], ALU.mult, [x_t, g_bc], [x_t])
        k.tt("dve", x_t[:], x_t[:], b_bc[:], ALU.add, [x_t, b_bc], [x_t])

    def phase_out(self, l, b):
        cfg, k, ps = self.cfg, self.k, self.ps
        L, C, NT, NB = cfg.L, cfg.C, cfg.NT, cfg.nb
        CAP, NSLOT = self.CAP, self.NSLOT
        with self.phase(f"out{l}_{b}") as ph:
            wo = ph.sb("wo", [128, KD, D], BF16)
            with self.phase(f"ldwo{l}_{b}") as p2:
                stage = [p2.sb(f"stg{i}", [128, D], F32) for i in range(2)]
                self.load_w_bf(p2, wo, self.w_out, l * D, stage)
            bc = {}
            for nm, r, j, one in (("m2", b, 2, False), ("m2c", NB, 2, False), ("s3", b, 3, False), ("s3c", NB, 3, False),
                                  ("s4", b, 4, True), ("s4c", NB, 4, True)):
                bc[nm] = ph.sb(nm, [128, D], F32)
                self.load_bc(bc[nm], l, r, j, one)
            lng = ph.sb("lng", [128, D], F32)
            lnb = ph.sb("lnb", [128, D], F32)
            k.dma("sp", lng[:], self.ln_g[l * 2:l * 2 + 1, :].partition_broadcast(128), [self.ln_g], [lng])
            k.dma("sp", lnb[:], self.ln_b[l * 2:l * 2 + 1, :].partition_broadcast(128), [self.ln_b], [lnb])
            wr = ph.sb("wr", [128, KD * 36], F32)
            k.dma("sp", wr[:], self.w_r[l * 128:(l + 1) * 128, :], [self.w_r], [wr])
            br = ph.sb("br", [128, 36], F32)
            k.dma("sp", br[:], self.b_r[l:l + 1, :].partition_broadcast(128), [self.b_r], [br])
            st3 = (ph.sb("stats", [128, 4, 6], F32), ph.sb("mv", [128, 2], F32), ph.sb("rstd", [128, 1], F32))
            zblk = ph.sb("zblk", [128, KD, 512], BF16)
            xt = ph.sb("xt", [128, D], F32)
            tm = ph.sb("tm", [128, D], F32)
            vt = ph.sb("vt", [128, D], F32)
            vb = ph.sb("vb", [128, D], BF16)
            vT = ph.sb("vT", [128, KD, 128], F32)
            sm = {nm: ph.sb(nm, shp, F32) for nm, shp in (
                ("lg", [128, 36]), ("gmax", [128, 1]), ("ngmax", [128, 1]), ("ohg", [128, 4]), ("e4", [128, 4]),
                ("pg", [128, 1]), ("prod", [128, 32]), ("lsel", [128, 8]), ("m8", [128, 8]), ("oh1", [128, 8]),
                ("oh2", [128, 8]), ("g1", [128, 1]), ("o32a", [128, 32]), ("o32b", [128, 32]), ("mask", [128, 32]),
                ("pos", [128, 32]), ("ovf", [128, 32]), ("slotf", [128, 2]), ("tmp32", [128, 32]))}
            if b == 0:
                k.op("dve", lambda e: e.memset(self.off[:], 0.0), [], [self.off])
            for t in range(NT):
                if t % 4 == 0:
                    n = min(512, L - t * 128)
                    k.dma("sp", zblk[:, :, 0:n], self.zT[:, t * 128:t * 128 + n].rearrange("(k p) n -> p k n", p=128), [self.zT], [zblk])
                isctx = t * 128 < C
                sfx = "c" if isctx else ""
                tl = slice((t % 4) * 128, (t % 4 + 1) * 128)
                k.dma("sp", xt[:], self.xs[b][t * 128:(t + 1) * 128, :], [self.xs[b]], [xt])
                for nb_ in range(4):
                    pst = ps[nb_]
                    for kc in range(KD):
                        k.mm(pst[:, :], zblk[:, kc, tl], wo[:, kc, nb_ * 512:(nb_ + 1) * 512], kc == 0, kc == KD - 1, [zblk, wo], [pst])
                    k.tt("dve", tm[:, nb_ * 512:(nb_ + 1) * 512], pst[:, :], bc["m2" + sfx][:, nb_ * 512:(nb_ + 1) * 512], ALU.mult,
                         [pst, bc["m2" + sfx]], [tm])
                k.stt(xt[:], xt[:], DN_ALPHA, tm[:], ALU.mult, ALU.add, [xt, tm], [xt])
                self.ln_affine(st3, xt, self.eps_ln, lng, lnb)
                k.dma("pool", self.xs[b][t * 128:(t + 1) * 128, :], xt[:], [xt], [self.xs[b]])
                self.ln_stats(st3, xt, self.eps_ada)
                k.ts("dve", vt[:], xt[:], st3[1][:, 0:1], st3[2][:, 0:1], ALU.subtract, ALU.mult, [xt, st3[1], st3[2]], [vt])
                k.tt("pool", vt[:], vt[:], bc["s4" + sfx][:], ALU.mult, [vt, bc["s4" + sfx]], [vt])
                k.tt("dve", vt[:], vt[:], bc["s3" + sfx][:], ALU.add, [vt, bc["s3" + sfx]], [vt])
                k.copy("pool", vb[:], vt[:], [vt], [vb])
                for g4 in range(4):
                    pst = ps[4 + g4 % 2]
                    for j in range(4):
                        kc = g4 * 4 + j
                        k.tr(pst[:, j * 128:(j + 1) * 128], vt[:, kc * 128:(kc + 1) * 128], self.identf[:], [vt, self.identf], [pst])
                    k.copy(self.alt(), vT[:, g4 * 4:(g4 + 1) * 4, :], pst[:, :].rearrange("p (j n) -> p j n", j=4), [pst], [vT])
                for kc in range(KD):
                    k.mm(ps[6][:, 0:36], vT[:, kc, :], wr[:, kc * 36:(kc + 1) * 36], kc == 0, kc == KD - 1, [vT, wr], [ps[6]])
                s = sm
                k.tt("dve", s["lg"][:], ps[6][:, 0:36], br[:], ALU.add, [ps[6], br], [s["lg"]])
                k.op("dve", lambda e: e.tensor_reduce(s["gmax"][:], s["lg"][:, 0:4], AX.X, ALU.max), [s["lg"]], [s["gmax"]])
                k.ts("dve", s["ngmax"][:], s["gmax"][:], -1.0, None, ALU.mult, None, [s["gmax"]], [s["ngmax"]])
                k.ts("dve", s["ohg"][:], s["lg"][:, 0:4], s["gmax"][:, 0:1], None, ALU.is_equal, None, [s["lg"], s["gmax"]], [s["ohg"]])
                k.act(s["e4"][:], s["lg"][:, 0:4], AF.Exp, [s["lg"], s["ngmax"]], [s["e4"]], bias=s["ngmax"][:], scale=1.0)
                k.op("dve", lambda e: e.tensor_reduce(s["pg"][:], s["e4"][:], AX.X, ALU.add), [s["e4"]], [s["pg"]])
                k.op("dve", lambda e: e.reciprocal(s["pg"][:], s["pg"][:]), [s["pg"]], [s["pg"]])
                k.tt("dve", s["prod"][:].rearrange("p (g e) -> p g e", g=4), s["lg"][:, 4:36].rearrange("p (g e) -> p g e", g=4),
                     s["ohg"][:].rearrange("p (g o) -> p g o", o=1).broadcast_to([128, 4, 8]), ALU.mult, [s["lg"], s["ohg"]], [s["prod"]])
                k.op("dve", lambda e: e.tensor_reduce(s["lsel"][:], s["prod"][:].rearrange("p (g e) -> p e g", g=4), AX.X, ALU.add),
                     [s["prod"]], [s["lsel"]])
                k.op("dve", lambda e: e.max(s["m8"][:], s["lsel"][:]), [s["lsel"]], [s["m8"]])
                k.ts("dve", s["oh1"][:], s["lsel"][:], s["m8"][:, 0:1], None, ALU.is_equal, None, [s["lsel"], s["m8"]], [s["oh1"]])
                k.ts("dve", s["oh2"][:], s["lsel"][:], s["m8"][:, 1:2], None, ALU.is_equal, None, [s["lsel"], s["m8"]], [s["oh2"]])
                k.tt("dve", s["g1"][:], s["m8"][:, 0:1], s["m8"][:, 1:2], ALU.subtract, [s["m8"]], [s["g1"]])
                k.act(s["g1"][:], s["g1"][:], AF.Sigmoid, [s["g1"]], [s["g1"]])
                tt_i = b * NT + t
                gs = self.gate_sb
                k.tt("dve", gs[:, 2 * tt_i:2 * tt_i + 1], s["pg"][:], s["g1"][:], ALU.mult, [s["pg"], s["g1"]], [gs])
                k.tt("dve", gs[:, 2 * tt_i + 1:2 * tt_i + 2], s["pg"][:], gs[:, 2 * tt_i:2 * tt_i + 1], ALU.subtract, [s["pg"], gs], [gs])
                for nm, oh in (("o32a", "oh1"), ("o32b", "oh2")):
                    k.tt("dve", s[nm][:].rearrange("p (g e) -> p g e", g=4),
                         s["ohg"][:].rearrange("p (g o) -> p g o", o=1).broadcast_to([128, 4, 8]),
                         s[oh][:].rearrange("p (o e) -> p o e", o=1).broadcast_to([128, 4, 8]), ALU.mult, [s["ohg"], s[oh]], [s[nm]])
                k.tt("dve", s["mask"][:], s["o32a"][:], s["o32b"][:], ALU.add, [s["o32a"], s["o32b"]], [s["mask"]])
                k.mm(ps[7][:, 0:32], self.triu[:], s["mask"][:], True, True, [self.triu, s["mask"]], [ps[7]])
                k.mm(ps[7][:, 32:64], self.ones_f[:], s["mask"][:], True, True, [self.ones_f, s["mask"]], [ps[7]])
                k.tt("dve", s["pos"][:], ps[7][:, 0:32], s["mask"][:], ALU.subtract, [ps[7], s["mask"]], [s["pos"]])
                k.tt("dve", s["pos"][:], s["pos"][:], self.off[:], ALU.add, [s["pos"], self.off], [s["pos"]])
                k.ts("dve", s["ovf"][:], s["pos"][:], float(CAP), float(1 << 24), ALU.is_ge, ALU.mult, [s["pos"]], [s["ovf"]])
                k.tt("dve", s["pos"][:], s["pos"][:], s["ovf"][:], ALU.add, [s["pos"], s["ovf"]], [s["pos"]])
                k.tt("dve", s["pos"][:], s["pos"][:], self.ecap[:], ALU.add, [s["pos"], self.ecap], [s["pos"]])
                for kk_, nm in ((0, "o32a"), (1, "o32b")):
                    k.tt("dve", s["tmp32"][:], s["pos"][:], s[nm][:], ALU.mult, [s["pos"], s[nm]], [s["tmp32"]])
                    k.op("dve", lambda e, kk_=kk_: e.tensor_reduce(s["slotf"][:, kk_:kk_ + 1], s["tmp32"][:], AX.X, ALU.add),
                         [s["tmp32"]], [s["slotf"]])
                k.copy("dve", self.slot_sb[:, 2 * tt_i:2 * tt_i + 2], s["slotf"][:], [s["slotf"]], [self.slot_sb])
                k.ts("dve", s["slotf"][:], s["slotf"][:], -float(NSLOT // 2), None, ALU.add, None, [s["slotf"]], [s["slotf"]])
                k.copy("dve", self.slotB_sb[:, 2 * tt_i:2 * tt_i + 2], s["slotf"][:], [s["slotf"]], [self.slotB_sb])
                k.tt("dve", self.off[:], self.off[:], ps[7][:, 32:64], ALU.add, [self.off, ps[7]], [self.off])
                for kk_ in range(2):
                    k.idma(self.xslots.ap(), bass.IndirectOffsetOnAxis(ap=self.slot_sb[:, 2 * tt_i + kk_:2 * tt_i + kk_ + 1], axis=0),
                           vb[:], None, [vb, self.slot_sb], [self.xslots], bounds_check=self.breg, oob_is_err=False)

    def phase_experts(self, l):
        cfg, k, ps = self.cfg, self.k, self.ps
        CAP = self.CAP
        groups = []
        s0 = 0
        while s0 < CAP:
            groups.append((s0, min(512, CAP - s0)))
            s0 += 512
        with self.phase(f"exp{l}") as ph:
            stage = [ph.sb(f"stg{i}", [128, 8192], F32) for i in range(2)]
            w1b = ph.sb("w1b", [128, KD, 512], BF16)
            w3b = ph.sb("w3b", [128, KD, 512], BF16)
            w2b = ph.sb("w2b", [128, 4, D], BF16)
            xb = [ph.sb(f"xb{i}", [128, D], BF16) for i in range(2)]
            xbT = ph.sb("xbT", [128, KD, 512], BF16)
            hT = ph.sb("hT", [128, 4, 512], BF16)
            h1 = [ph.sb(f"h1{i}", [128, 512], F32) for i in range(2)]
            yt = [ph.sb(f"yt{i}", [128, D], F32) for i in range(2)]
            si = 0
            for e in range(32):
                r0 = (l * 32 + e) * 128
                for (wsrc, wdst) in ((self.w1r, w1b), (self.w3r, w3b), (self.w2r, w2b)):
                    st = stage[si % 2]
                    k.dma("sp" if si % 2 == 0 else "sp", st[:], wsrc[r0:r0 + 128, :], [wsrc], [st])
                    wflat = wdst[:].rearrange("p a n -> p (a n)")
                    k.copy("dve", wflat[:, 0:4096], st[:, 0:4096], [st], [wdst])
                    k.copy("pool", wflat[:, 4096:8192], st[:, 4096:8192], [st], [wdst])
                    si += 1
                for (g0, gn) in groups:
                    nst = gn // 128
                    for st_ in range(nst):
                        x_b = xb[st_ % 2]
                        row = e * CAP + g0 + st_ * 128
                        k.dma("sp", x_b[:], self.xslots[row:row + 128, :], [self.xslots], [x_b])
                        for g4 in range(4):
                            pst = ps[4 + (st_ * 4 + g4) % 4]
                            pv = pst.ap().bitcast(BF16)
                            for j in range(4):
                                kc = g4 * 4 + j
                                k.tr(pv[:, j * 128:(j + 1) * 128], x_b[:, kc * 128:(kc + 1) * 128], self.identb[:], [x_b, self.identb], [pst])
                            k.copy(self.alt(), xbT[:, g4 * 4:(g4 + 1) * 4, st_ * 128:(st_ + 1) * 128],
                                   pv[:, 0:512].rearrange("p (j n) -> p j n", j=4), [pst], [xbT])
                    for f in range(4):
                        p1, p3 = ps[(f % 2) * 2], ps[(f % 2) * 2 + 1]
                        for kc in range(KD):
                            k.mm(p1[:, 0:gn], w1b[:, kc, f * 128:(f + 1) * 128], xbT[:, kc, 0:gn], kc == 0, kc == KD - 1, [w1b, xbT], [p1])
                        for kc in range(KD):
                            k.mm(p3[:, 0:gn], w3b[:, kc, f * 128:(f + 1) * 128], xbT[:, kc, 0:gn], kc == 0, kc == KD - 1, [w3b, xbT], [p3])
                        hh = h1[f % 2]
                        k.act(hh[:, 0:gn], p1[:, 0:gn], AF.Silu, [p1], [hh])
                        k.tt("dve", hT[:, f, 0:gn], hh[:, 0:gn], p3[:, 0:gn], ALU.mult, [hh, p3], [hT])
                    for st_ in range(nst):
                        y_t = yt[st_ % 2]
                        for nb_ in range(4):
                            pst = ps[4 + nb_]
                            for f in range(4):
                                k.mm(pst[:, :], hT[:, f, st_ * 128:(st_ + 1) * 128], w2b[:, f, nb_ * 512:(nb_ + 1) * 512], f == 0, f == 3,
                                     [hT, w2b], [pst])
                            k.copy(self.alt(), y_t[:, nb_ * 512:(nb_ + 1) * 512], pst[:, :], [pst], [y_t])
                        row = e * CAP + g0 + st_ * 128
                        ys = self.yslots[row // (self.NSLOT // 2)]
                        row = row % (self.NSLOT // 2)
                        k.dma("pool", ys[row:row + 128, :], y_t[:], [y_t], [ys])

    def phase_combine(self, l, b):
        cfg, k, ps = self.cfg, self.k, self.ps
        L, C, S, NT, NB = cfg.L, cfg.C, cfg.S, cfg.NT, cfg.nb
        last = l == cfg.depth - 1
        with self.phase(f"cmb{l}_{b}") as ph:
            bc = {}
            for nm, r in (("m5", b), ("m5c", NB)):
                bc[nm] = ph.sb(nm, [128, D], F32)
                self.load_bc(bc[nm], l, r, 5, False)
            lng = ph.sb("lng", [128, D], F32)
            lnb = ph.sb("lnb", [128, D], F32)
            k.dma("sp", lng[:], self.ln_g[l * 2 + 1:l * 2 + 2, :].partition_broadcast(128), [self.ln_g], [lng])
            k.dma("sp", lnb[:], self.ln_b[l * 2 + 1:l * 2 + 2, :].partition_broadcast(128), [self.ln_b], [lnb])
            st3 = (ph.sb("stats", [128, 4, 6], F32), ph.sb("mv", [128, 2], F32), ph.sb("rstd", [128, 1], F32))
            y0 = [ph.sb(f"y0{i}", [128, D], F32) for i in range(2)]
            y1 = [ph.sb(f"y1{i}", [128, D], F32) for i in range(2)]
            xt = [ph.sb(f"xt{i}", [128, D], F32) for i in range(2)]
            for t in range(NT):
                isctx = t * 128 < C
                if last and isctx:
                    continue
                tt_i = b * NT + t
                ya, yb_, x_t = y0[t % 2], y1[t % 2], xt[t % 2]
                for kk_, yy in ((0, ya), (1, yb_)):
                    k.op("pool", lambda e, yy=yy: e.memset(yy[:], 0.0), [], [yy])
                    k.idma(yy[:], None, self.yslots[0].ap(),
                           bass.IndirectOffsetOnAxis(ap=self.slot_sb[:, 2 * tt_i + kk_:2 * tt_i + kk_ + 1], axis=0),
                           [self.yslots[0], self.slot_sb], [yy], bounds_check=self.breg_h, oob_is_err=False)
                    k.idma(yy[:], None, self.yslots[1].ap(),
                           bass.IndirectOffsetOnAxis(ap=self.slotB_sb[:, 2 * tt_i + kk_:2 * tt_i + kk_ + 1], axis=0),
                           [self.yslots[1], self.slotB_sb], [yy], bounds_check=self.breg_h, oob_is_err=False)
                k.dma("sp", x_t[:], self.xs[b][t * 128:(t + 1) * 128, :], [self.xs[b]], [x_t])
                k.ts("dve", ya[:], ya[:], self.gate_sb[:, 2 * tt_i:2 * tt_i + 1], None, ALU.mult, None, [ya, self.gate_sb], [ya])
                k.stt(ya[:], yb_[:], self.gate_sb[:, 2 * tt_i + 1:2 * tt_i + 2], ya[:], ALU.mult, ALU.add, [yb_, self.gate_sb, ya], [ya])
                k.tt("pool", ya[:], ya[:], bc["m5c" if isctx else "m5"][:], ALU.mult, [ya, bc["m5c" if isctx else "m5"]], [ya])
                k.stt(x_t[:], x_t[:], DN_ALPHA, ya[:], ALU.mult, ALU.add, [x_t, ya], [x_t])
                self.ln_affine(st3, x_t, self.eps_ln, lng, lnb)
                if last:
                    r0 = b * S + t * 128 - C
                    k.dma("pool", self.out[r0:r0 + 128, :], x_t[:], [x_t], [self.out])
                else:
                    k.dma("pool", self.xs[b][t * 128:(t + 1) * 128, :], x_t[:], [x_t], [self.xs[b]])

    def finish(self):
        self.k.barrier()
        return self


def prep_shared(cfg, inputs):
    DEP = cfg.depth
    sh = {}
    w_mod = np.asarray(inputs["w_mod"])[:DEP]
    sh["w_mod"] = np.ascontiguousarray(
        w_mod.reshape(DEP, KD, 128, 24, 512).transpose(0, 3, 2, 1, 4)).reshape(DEP * 24 * 128, KD * 512)
    sh["b_mod"] = np.ascontiguousarray(np.asarray(inputs["b_mod"])[:DEP])
    w_in = np.asarray(inputs["w_in"])[:DEP]
    w_e = w_in[:, :, EXT_COLS]
    sh["w_ext"] = np.ascontiguousarray(
        w_e.reshape(DEP, KD, 128, NT_EXT, 128).transpose(0, 3, 2, 1, 4)).reshape(DEP * NT_EXT * 128, KD * 128)
    sh["ident_f"] = np.eye(128, dtype=np.float32)
    S, C, L = cfg.S, cfg.C, cfg.L
    pos = np.arange(S)
    rowp = (pos // 64).astype(np.float32)
    colp = (pos % 64).astype(np.float32)
    inv = (np.float32(10000.0) ** (-np.arange(16, dtype=np.float32) / np.float32(16))).astype(np.float32)
    d = np.arange(64)
    axis, half, f = d // 32, (d % 32) // 16, d % 16
    ang = np.where(axis[:, None] == 0, rowp[None, :], colp[None, :]).astype(np.float32) * inv[f][:, None]
    cosT = np.ones((64, L), np.float32)
    sinT = np.zeros((64, L), np.float32)
    cosT[:, C:] = np.cos(ang)
    sinT[:, C:] = np.sin(ang) * np.where(half == 0, -1.0, 1.0)[:, None]
    sh["ropec"] = np.ascontiguousarray(np.concatenate([cosT, cosT], 0).astype(np.float32))
    sh["ropes"] = np.ascontiguousarray(np.concatenate([sinT, sinT], 0).astype(np.float32))
    kk_, qq_ = np.arange(128)[:, None], np.arange(128)[None, :]
    sh["maskP"] = (qq_ <= kk_).astype(np.float32)
    sh["maskN"] = (kk_ <= qq_).astype(np.float32)
    def chT(v, nt):
        return np.asarray(v).reshape(nt, 128).T
    rv = np.zeros((DEP, 128, 96), np.float32)
    for l in range(DEP):
        mu = np.asarray(inputs["rwkv_mu"])[l]
        rv[l, :, 0:21] = chT(mu[0], 21)
        rv[l, :, 21:42] = chT(mu[1], 21)
        w0 = np.asarray(inputs["rwkv_w0"])[l]
        a0 = np.asarray(inputs["rwkv_a0"])[l]
        kv = np.asarray(inputs["rwkv_kvec"])[l]
        lnx = np.asarray(inputs["rwkv_lnx"])[l]
        for d_ in range(2):
            rv[l, :, 42 + d_ * 6:48 + d_ * 6] = chT(w0[d_], 6)
            rv[l, :, 54 + d_ * 6:60 + d_ * 6] = chT(a0[d_], 6)
        rv[l, :, 66:72] = chT(kv[0], 6)
        rv[l, :, 72:78] = chT(kv[1], 6)
        rv[l, :, 78:84] = chT(kv[2], 6)
        rv[l, :, 84:90] = chT(lnx[0], 6)
        rv[l, :, 90:96] = chT(lnx[1], 6)
    sh["rwvec"] = rv.reshape(DEP * 128, 96)
    sh["w_up"] = np.ascontiguousarray(np.asarray(inputs["rwkv_w_up"])[:DEP].reshape(DEP * 128, 768))
    sh["a_up"] = np.ascontiguousarray(np.asarray(inputs["rwkv_a_up"])[:DEP].reshape(DEP * 128, 768))
    sh["g_up"] = np.ascontiguousarray(np.asarray(inputs["rwkv_g_up"])[:DEP].reshape(DEP * 128, 768))
    s_, t_ = np.arange(64)[:, None], np.arange(64)[None, :]
    MS, MI = (s_ < t_).astype(np.float32), (s_ <= t_).astype(np.float32)
    mA_f = np.concatenate([MS, MS, MS.T, MS.T, MS, MS, MI, MI], axis=1)
    mA_b = np.concatenate([MS.T, MS.T, MS, MS, MS.T, MS.T, MI.T, MI.T], axis=1)
    sh["maskA"] = np.ascontiguousarray(np.concatenate([mA_f, mA_f, mA_b, mA_b], axis=0))
    mB_f = -np.concatenate([MI, MI], axis=1)
    mB_b = -np.concatenate([MI.T, MI.T], axis=1)
    sh["maskB"] = np.ascontiguousarray(np.concatenate([mB_f, mB_f, mB_b, mB_b], axis=0))
    e64 = np.eye(64, dtype=np.float32)
    sh["ident2"] = np.ascontiguousarray(np.tile(np.concatenate([e64, e64], axis=1), (2, 1)))
    bo = np.zeros((128, 128), np.float32); bo[:64, :64] = 1; bo[64:, 64:] = 1
    sh["bones"] = bo
    rm = np.ones((128, L), np.float32); rm[:, ::64] = 0
    sh["resetm"] = rm
    sh["w_branch"] = np.ascontiguousarray(np.asarray(inputs["w_branch"])[:DEP].reshape(DEP * D, D))
    sh["w_out"] = np.ascontiguousarray(np.asarray(inputs["w_out"])[:DEP].reshape(DEP * D, D))
    sh["ln_g"] = np.ascontiguousarray(np.asarray(inputs["ln_g"])[:DEP].reshape(DEP * 2, D))
    sh["ln_b"] = np.ascontiguousarray(np.asarray(inputs["ln_b"])[:DEP].reshape(DEP * 2, D))
    wr_ = np.concatenate([np.asarray(inputs["w_rg"])[:DEP], np.asarray(inputs["w_re"])[:DEP]], axis=2)
    sh["w_r"] = np.ascontiguousarray(wr_.reshape(DEP, KD, 128, 36).transpose(0, 2, 1, 3)).reshape(DEP * 128, KD * 36)
    sh["b_r"] = np.ascontiguousarray(np.concatenate([np.asarray(inputs["b_rg"])[:DEP], np.asarray(inputs["b_re"])[:DEP]], axis=1))
    for nm, src_ in (("w1r", "w1"), ("w3r", "w3")):
        w = np.asarray(inputs[src_])[:DEP]
        sh[nm] = np.ascontiguousarray(w.reshape(DEP, 32, KD, 128, 512).transpose(0, 1, 3, 2, 4)).reshape(DEP * 32 * 128, 8192)
    w = np.asarray(inputs["w2"])[:DEP]
    sh["w2r"] = np.ascontiguousarray(w.reshape(DEP, 32, 4, 128, D).transpose(0, 1, 3, 2, 4)).reshape(DEP * 32 * 128, 8192)
    sh["triu"] = (np.arange(128)[:, None] <= np.arange(128)[None, :]).astype(np.float32)
    sh["ecap"] = np.tile((np.arange(32, dtype=np.float32) * cfg.cap)[None, :], (128, 1)).astype(np.float32)
    sh["diff_lam"] = np.ascontiguousarray(np.asarray(inputs["diff_lam"])[:DEP].reshape(DEP, 256))
    sh["diff_subln"] = np.ascontiguousarray(np.asarray(inputs["diff_subln"])[:DEP])
    sh["win_sink"] = np.ascontiguousarray(np.asarray(inputs["win_sink"])[:DEP])
    return sh


def prep_core(cfg, inputs, core):
    NB, S, C = cfg.nb, cfg.S, cfg.C
    b0 = core * NB
    m = {}
    m["xin"] = np.ascontiguousarray(np.asarray(inputs["x"])[b0:b0 + NB].reshape(NB * S, D))
    m["ctxin"] = np.ascontiguousarray(np.asarray(inputs["ctx"])[b0:b0 + NB].reshape(NB * C, D))
    cc = np.concatenate([np.asarray(inputs["c"])[b0:b0 + NB], np.asarray(inputs["c_ctx"])[None, :]], axis=0)
    NR = NB + 1
    m["ccT"] = np.ascontiguousarray(cc.reshape(NR, KD, 128).transpose(2, 1, 0)).reshape(128, KD * NR)
    return m


def run(cfg, inputs, trace=False):
    prog = Prog(cfg).build()
    shared = prep_shared(cfg, inputs)
    in_maps = []
    for c in range(cfg.ncores):
        m = dict(shared)
        m.update(prep_core(cfg, inputs, c))
        in_maps.append({n: m[n] for n in prog.inputs})
    res = run_bass_kernel_spmd(prog.nc, in_maps, core_ids=list(range(cfg.ncores)), trace=trace)
    return prog, res


N_CORES = 4


def kernel(**inputs):
    nb = 16 // N_CORES
    cap = int(math.ceil(3.1 * 2 * nb * (2048 + 256) / 32 / 128.0)) * 128
    cfg = Cfg(ncores=N_CORES, nb=nb, S=2048, C=256, depth=4, cap=cap)
    prog, res = run(cfg, inputs)
    outs = [res.results[c]["out"].reshape(cfg.nb, cfg.S, D) for c in range(cfg.ncores)]
    return np.concatenate(outs, axis=0).astype(np.float32)
```
